# Optimizing a Trainium2 kernel written in Bass

```python
import math
import jax
import jax.numpy as jnp
from jax import lax
import numpy as np

D_MODEL = 2048
BATCH = 2
SEQ = 8192
DEPTH = 1

GRID_W = 64
CTX_LEN = 256
EPS = 1e-6

D_MIX = D_MODEL
M_HEADS = 4
M_DV = D_MIX // (2 * M_HEADS)
M_DQK = M_DV // 2
M_V = M_HEADS * M_DV
M_QK = M_HEADS * M_DQK
M_GATES = 2 * 2 * M_HEADS
GATE_SOFTCAP = 15.0
G_HEADS = 8
G_DK = D_MIX // (2 * G_HEADS)
G_W = G_HEADS * G_DK
CONV_K = 5

CHUNK = 64

IN_SPLITS = (M_QK, M_QK, M_V, M_V, M_GATES, 3 * G_W, G_W, 2 * G_HEADS, 2 * G_HEADS)
D_IN = 2 * M_QK + 2 * M_V + M_GATES + 4 * G_W + 4 * G_HEADS

N_GROUPS = 4
EXPERTS_PER_GROUP = 8
N_EXPERTS = N_GROUPS * EXPERTS_PER_GROUP
TOP_K = 2
D_EXPERT = 3 * D_MODEL // 8
MOE_BLOCK = 128

kernel_name = 'hymba_mlstm_gdn_hmoe_prefix_dit'

F32 = jnp.float32


def rmsnorm(x, g):
    xf = x.astype(F32)
    y = xf * lax.rsqrt(jnp.mean(xf * xf, axis=-1, keepdims=True) + EPS)
    return (y * g.astype(F32)).astype(x.dtype)


def modulate(h, shift, scale):
    return h * (1.0 + scale) + shift


def split_cols(z):
    parts, off = [], 0
    for w in IN_SPLITS:
        parts.append(z[..., off:off + w])
        off += w
    return parts


def to_heads(t, n_heads):
    b, n, _ = t.shape
    return t.reshape(b, n, n_heads, -1).transpose(0, 2, 1, 3)


def from_heads_norm(h, g):
    b, nh, n, d = h.shape
    h = rmsnorm(h.transpose(0, 2, 1, 3), g.reshape(nh, d))
    return h.reshape(b, n, nh * d)


def to_chunks(t):
    return t.reshape(t.shape[:2] + (t.shape[2] // CHUNK, CHUNK) + t.shape[3:])


def from_chunks(t):
    return t.reshape(t.shape[:2] + (t.shape[2] * t.shape[3],) + t.shape[4:])


def along(t, d):
    return t if d == 0 else jnp.flip(t, axis=2)


def dwconv_lines(u, w):
    pad = CONV_K // 2
    return lax.conv_general_dilated(u, w[:, None, :].astype(u.dtype), (1,), [(pad, pad)],
                                    dimension_numbers=('NWC', 'WIO', 'NWC'),
                                    feature_group_count=u.shape[-1])


def mlstm_chunk_states(k, v, li, lf, state0):
    b = jnp.cumsum(lf, axis=-1)
    b_last = b[..., -1]
    a = b_last[..., None] - b + li
    a_max = jnp.max(a, axis=-1)

    def step(carry, xs):
        c_st, n_st, m_st = carry
        k_c, v_c, bl, a_c, am = xs
        m_new = jnp.maximum(bl + m_st, am)
        decay = jnp.exp(bl + m_st - m_new)
        w = jnp.exp(a_c - m_new[..., None])
        c_new = decay[..., None, None] * c_st + jnp.einsum('bhlv,bhlk->bhvk', v_c * w[..., None], k_c)
        n_new = decay[..., None] * n_st + jnp.einsum('bhl,bhlk->bhk', w, k_c)
        return (c_new, n_new, m_new), (c_st, n_st, m_st)

    xs = tuple(jnp.moveaxis(t, 2, 0) for t in (k, v, b_last, a, a_max))
    final, starts = lax.scan(step, state0, xs)
    starts = tuple(jnp.moveaxis(t, 0, 2) for t in starts)
    return final, starts, b


def mlstm_chunk_outputs(q, k, v, li, b, starts):
    c0, n0, m0 = starts
    lower = jnp.tril(jnp.ones((CHUNK, CHUNK), dtype=bool))
    dmat = jnp.where(lower, b[..., :, None] - b[..., None, :] + li[..., None, :], -jnp.inf)
    inter = b + m0[..., None]
    m = jnp.maximum(jnp.max(dmat, axis=-1), inter)
    s = jnp.einsum('bhcld,bhcsd->bhcls', q, k) * jnp.exp(dmat - m[..., None])
    e = jnp.exp(inter - m)
    num = jnp.einsum('bhcls,bhcsv->bhclv', s, v) + e[..., None] * jnp.einsum('bhcld,bhcvd->bhclv', q, c0)
    den = jnp.sum(s, axis=-1) + e * jnp.einsum('bhcld,bhcd->bhcl', q, n0)
    return num / jnp.maximum(jnp.abs(den), jnp.exp(-m))[..., None]


def mlstm_run(q, k, v, li, lf, state0, with_out):
    kc, vc, lic = to_chunks(k), to_chunks(v), to_chunks(li)
    final, starts, b = mlstm_chunk_states(kc, vc, lic, to_chunks(lf), state0)
    if not with_out:
        return None, final
    return from_chunks(mlstm_chunk_outputs(to_chunks(q), kc, vc, lic, b, starts)), final


def mlstm_prep(q, k, v, gates, b_gate):
    bsz, n, _ = q.shape
    q = to_heads(q, M_HEADS).astype(F32) * (M_DQK ** -0.5)
    k = to_heads(k, M_HEADS).astype(F32)
    v = to_heads(v, M_HEADS).astype(F32)
    pre = gates.astype(F32).reshape(bsz, n, 2, 2, M_HEADS) + b_gate.astype(F32)
    pre = GATE_SOFTCAP * jnp.tanh(pre / GATE_SOFTCAP)
    pre = jnp.moveaxis(pre, 1, -1)
    return q, k, v, pre[:, :, 0], jax.nn.log_sigmoid(pre[:, :, 1])


def mlstm_group(zc, zl, b_gate, g_head, with_ctx):
    qc, kc, vc, lic, lfc = mlstm_prep(zc[0], zc[1], zc[2], zc[4], b_gate)
    ql, kl, vl, lil, lfl = mlstm_prep(zl[0], zl[1], zl[2], zl[4], b_gate)
    bsz = ql.shape[0]
    hl, hc = 0.0, 0.0
    for d in range(2):
        s0 = (jnp.zeros((bsz, M_HEADS, M_DV, M_DQK), F32), jnp.zeros((bsz, M_HEADS, M_DQK), F32),
              jnp.zeros((bsz, M_HEADS), F32))
        out_c, fin_c = mlstm_run(along(qc, d), along(kc, d), along(vc, d), along(lic[:, d], d),
                                 along(lfc[:, d], d), s0, with_ctx)
        out_l, _ = mlstm_run(along(ql, d), along(kl, d), along(vl, d), along(lil[:, d], d),
                             along(lfl[:, d], d), fin_c, True)
        hl = hl + along(out_l, d)
        if with_ctx:
            hc = hc + along(out_c, d)
    yl = from_heads_norm(hl, g_head) * jax.nn.sigmoid(zl[3].astype(F32))
    yc = from_heads_norm(hc, g_head) * jax.nn.sigmoid(zc[3].astype(F32)) if with_ctx else None
    return yl, yc


def l2norm(t):
    return t * lax.rsqrt(jnp.sum(t * t, axis=-1, keepdims=True) + EPS)


def gdn_chunk_prep(k, v, beta, g):
    gcum = jnp.cumsum(g, axis=-1)
    strict = jnp.tril(jnp.ones((CHUNK, CHUNK), dtype=bool), -1)
    diff = jnp.where(strict, gcum[..., :, None] - gcum[..., None, :], -jnp.inf)
    a_mat = beta[..., :, None] * jnp.einsum('bhcid,bhcjd->bhcij', k, k) * jnp.exp(diff)
    t_mat = a_mat + jnp.eye(CHUNK, dtype=F32)
    rhs = jnp.concatenate([v * beta[..., None], k * (beta * jnp.exp(gcum))[..., None]], axis=-1)
    sol = lax.linalg.triangular_solve(t_mat, rhs, left_side=True, lower=True, unit_diagonal=True)
    dv = v.shape[-1]
    return gcum, sol[..., :dv], sol[..., dv:]


def gdn_chunk_states(k, u, w, gcum, s0):
    g_last = gcum[..., -1]
    kd = k * jnp.exp(g_last[..., None] - gcum)[..., None]

    def step(s_st, xs):
        kd_c, u_c, w_c, gl = xs
        v_new = u_c - jnp.einsum('bhlk,bhkv->bhlv', w_c, s_st)
        s_new = s_st * jnp.exp(gl)[..., None, None] + jnp.einsum('bhlk,bhlv->bhkv', kd_c, v_new)
        return s_new, (s_st, v_new)

    xs = tuple(jnp.moveaxis(t, 2, 0) for t in (kd, u, w, g_last))
    final, (starts, vnew) = lax.scan(step, s0, xs)
    return final, jnp.moveaxis(starts, 0, 2), jnp.moveaxis(vnew, 0, 2)


def gdn_chunk_outputs(q, k, gcum, starts, vnew):
    lower = jnp.tril(jnp.ones((CHUNK, CHUNK), dtype=bool))
    diff = jnp.where(lower, gcum[..., :, None] - gcum[..., None, :], -jnp.inf)
    attn = jnp.einsum('bhcid,bhcjd->bhcij', q, k) * jnp.exp(diff)
    inter = jnp.einsum('bhclk,bhckv->bhclv', q * jnp.exp(gcum)[..., None], starts)
    return inter + jnp.einsum('bhcij,bhcjv->bhciv', attn, vnew)


def gdn_run(q, k, v, beta, g, s0, with_out):
    kc = to_chunks(k)
    gcum, u, w = gdn_chunk_prep(kc, to_chunks(v), to_chunks(beta), to_chunks(g))
    final, starts, vnew = gdn_chunk_states(kc, u, w, gcum, s0)
    if not with_out:
        return None, final
    return from_chunks(gdn_chunk_outputs(to_chunks(q), kc, gcum, starts, vnew)), final


def gdn_prep(qkv, a, b, a_log, dt_bias):
    bsz, n, _ = qkv.shape
    qkv = jax.nn.silu(qkv.astype(F32))
    q = l2norm(to_heads(qkv[..., :G_W], G_HEADS)) * (G_DK ** -0.5)
    k = l2norm(to_heads(qkv[..., G_W:2 * G_W], G_HEADS))
    v = to_heads(qkv[..., 2 * G_W:], G_HEADS)
    a = a.astype(F32).reshape(bsz, n, 2, G_HEADS)
    g = -jnp.exp(a_log.astype(F32)) * jax.nn.softplus(a + dt_bias.astype(F32))
    beta = jax.nn.sigmoid(b.astype(F32).reshape(bsz, n, 2, G_HEADS))
    return q, k, v, jnp.moveaxis(beta, 1, -1), jnp.moveaxis(g, 1, -1)


def gdn_group(zc, zl, a_log, dt_bias, w_conv, g_head, with_ctx):
    bsz, n, cq = zl[0].shape
    rows = n // GRID_W
    qkv_l = dwconv_lines(zl[0].reshape(bsz * rows, GRID_W, cq), w_conv).reshape(bsz, n, cq)
    qkv_c = dwconv_lines(zc[0], w_conv)
    qc, kc, vc, bc, gdc = gdn_prep(qkv_c, zc[2], zc[3], a_log, dt_bias)
    ql, kl, vl, bl, gdl = gdn_prep(qkv_l, zl[2], zl[3], a_log, dt_bias)
    ol, oc = 0.0, 0.0
    for d in range(2):
        s0 = jnp.zeros((bsz, G_HEADS, G_DK, G_DK), F32)
        out_c, fin_c = gdn_run(along(qc, d), along(kc, d), along(vc, d), along(bc[:, d], d),
                               along(gdc[:, d], d), s0, with_ctx)
        out_l, _ = gdn_run(along(ql, d), along(kl, d), along(vl, d), along(bl[:, d], d),
                           along(gdl[:, d], d), fin_c, True)
        ol = ol + along(out_l, d)
        if with_ctx:
            oc = oc + along(out_c, d)
    yl = from_heads_norm(ol, g_head) * jax.nn.silu(zl[1].astype(F32))
    yc = from_heads_norm(oc, g_head) * jax.nn.silu(zc[1].astype(F32)) if with_ctx else None
    return yl, yc


def token_mixing(hc, hl, w_in, b_gate_m, a_log, dt_bias, w_conv, g_head_m, g_head_d, w_out, with_ctx):
    zc = split_cols(hc @ w_in)
    zl = split_cols(hl @ w_in)
    yml, ymc = mlstm_group(zc[:5], zl[:5], b_gate_m, g_head_m, with_ctx)
    ydl, ydc = gdn_group(zc[5:], zl[5:], a_log, dt_bias, w_conv, g_head_d, with_ctx)
    odt = hl.dtype
    yl = jnp.concatenate([yml, ydl], axis=-1).astype(odt) @ w_out
    yc = (jnp.concatenate([ymc, ydc], axis=-1).astype(odt) @ w_out) if with_ctx else None
    return yl, yc


def hier_moe(h, w_grp, b_grp, w_rtr, b_rtr, w1, w3, w2):
    n_tok, d = h.shape
    grp_prob = jax.nn.softmax((h @ w_grp).astype(F32) + b_grp.astype(F32), axis=-1)
    p_grp, grp = lax.top_k(grp_prob, 1)
    exp_logits = ((h @ w_rtr).astype(F32) + b_rtr.astype(F32)).reshape(n_tok, N_GROUPS, EXPERTS_PER_GROUP)
    in_grp = exp_logits[jnp.arange(n_tok), grp[:, 0]]
    top_p, top_e = lax.top_k(jax.nn.softmax(in_grp, axis=-1), TOP_K)
    wts = p_grp * top_p / jnp.sum(top_p, axis=-1, keepdims=True)
    eid = grp * EXPERTS_PER_GROUP + top_e

    n_asg = n_tok * TOP_K
    e_flat = eid.reshape(n_asg)
    tok_flat = jnp.repeat(jnp.arange(n_tok, dtype=jnp.int32), TOP_K)
    w_flat = wts.reshape(n_asg)
    order = jnp.argsort(e_flat)
    e_s, tok_s, w_s = e_flat[order], tok_flat[order], w_flat[order]
    counts = jnp.bincount(e_flat, length=N_EXPERTS)
    start = jnp.cumsum(counts) - counts
    padded = (counts + MOE_BLOCK - 1) // MOE_BLOCK * MOE_BLOCK
    pend = jnp.cumsum(padded)
    dest = pend[e_s] - padded[e_s] + jnp.arange(n_asg) - start[e_s]
    n_blk = -(-n_asg // MOE_BLOCK) + N_EXPERTS
    n_rows = n_blk * MOE_BLOCK
    row_tok = jnp.full((n_rows,), n_tok, jnp.int32).at[dest].set(tok_s)
    row_w = jnp.zeros((n_rows,), F32).at[dest].set(w_s)
    blk_e = jnp.minimum(jnp.searchsorted(pend, jnp.arange(n_blk) * MOE_BLOCK, side='right'), N_EXPERTS - 1)
    h_pad = jnp.concatenate([h, jnp.zeros((1, d), h.dtype)], axis=0)
    xb = h_pad[row_tok].reshape(n_blk, MOE_BLOCK, d)

    def expert_block(args):
        xblk, e = args
        return (jax.nn.silu(xblk @ w1[e]) * (xblk @ w3[e])) @ w2[e]

    yb = lax.map(expert_block, (xb, blk_e)).reshape(n_rows, d)
    out = jnp.zeros((n_tok + 1, d), F32).at[row_tok].add(yb.astype(F32) * row_w[:, None])
    return out[:n_tok].astype(h.dtype)


def setup_inputs(seed: int = 0) -> dict:
    key = jax.random.key(seed)
    ks = jax.random.split(key, 32)

    def nrm(k, shape, scale):
        return jax.random.normal(k, shape, F32) * scale

    b_i = nrm(ks[9], (DEPTH, 2, 1, M_HEADS), 0.1)
    b_f = jnp.linspace(3.0, 6.0, M_HEADS, dtype=F32).reshape(1, 1, 1, M_HEADS) + nrm(ks[10], (DEPTH, 2, 1, M_HEADS), 0.1)
    a_decay = jax.random.uniform(ks[11], (DEPTH, 2, G_HEADS), F32, minval=1.0, maxval=16.0)
    dt = jnp.exp(jax.random.uniform(ks[12], (DEPTH, 2, G_HEADS), F32,
                                    minval=math.log(1e-3), maxval=math.log(1e-1)))
    return {
        'x': nrm(ks[0], (BATCH, SEQ, D_MODEL), 1.0),
        'c': nrm(ks[1], (BATCH, D_MODEL), 1.0),
        'ctx': nrm(ks[2], (BATCH, CTX_LEN, D_MODEL), 1.0),
        'c_ctx': nrm(ks[3], (D_MODEL,), 1.0),
        'w_ada': nrm(ks[4], (DEPTH, D_MODEL, 6 * D_MODEL), 0.01),
        'b_ada': nrm(ks[5], (DEPTH, 6 * D_MODEL), 0.02),
        'g_norm1': 1.0 + nrm(ks[6], (DEPTH, D_MODEL), 0.02),
        'g_norm2': 1.0 + nrm(ks[7], (DEPTH, D_MODEL), 0.02),
        'w_in': nrm(ks[8], (DEPTH, D_MODEL, D_IN), D_MODEL ** -0.5),
        'b_gate_m': jnp.concatenate([b_i, b_f], axis=2),
        'a_log': jnp.log(a_decay),
        'dt_bias': dt + jnp.log(-jnp.expm1(-dt)),
        'w_conv': nrm(ks[13], (DEPTH, CONV_K, 3 * G_W), CONV_K ** -0.5),
        'g_head_m': 1.0 + nrm(ks[14], (DEPTH, M_V), 0.02),
        'g_head_d': 1.0 + nrm(ks[15], (DEPTH, G_W), 0.02),
        'w_out': nrm(ks[16], (DEPTH, D_MIX, D_MODEL), D_MIX ** -0.5),
        'w_grp': nrm(ks[17], (DEPTH, D_MODEL, N_GROUPS), D_MODEL ** -0.5),
        'b_grp': nrm(ks[18], (DEPTH, N_GROUPS), 0.01),
        'w_rtr': nrm(ks[19], (DEPTH, D_MODEL, N_EXPERTS), D_MODEL ** -0.5),
        'b_rtr': nrm(ks[20], (DEPTH, N_EXPERTS), 0.01),
        'w1': nrm(ks[21], (DEPTH, N_EXPERTS, D_MODEL, D_EXPERT), D_MODEL ** -0.5),
        'w3': nrm(ks[22], (DEPTH, N_EXPERTS, D_MODEL, D_EXPERT), D_MODEL ** -0.5),
        'w2': nrm(ks[23], (DEPTH, N_EXPERTS, D_EXPERT, D_MODEL), D_EXPERT ** -0.5),
        'g_final': 1.0 + nrm(ks[24], (D_MODEL,), 0.02),
    }


def reference(x, c, ctx, c_ctx, w_ada, b_ada, g_norm1, g_norm2, w_in, b_gate_m, a_log, dt_bias,
              w_conv, g_head_m, g_head_d, w_out, w_grp, b_grp, w_rtr, b_rtr, w1, w3, w2, g_final):
    cx = ctx
    for l in range(DEPTH):
        last = l == DEPTH - 1
        ada = (jax.nn.silu(c) @ w_ada[l] + b_ada[l])[:, None, :]
        ada_c = (jax.nn.silu(c_ctx) @ w_ada[l] + b_ada[l])[None, None, :]
        sh1, sc1, gt1, sh2, sc2, gt2 = jnp.split(ada, 6, axis=-1)
        csh1, csc1, cgt1, csh2, csc2, cgt2 = jnp.split(ada_c, 6, axis=-1)

        hl = modulate(rmsnorm(x, g_norm1[l]), sh1, sc1)
        hc = modulate(rmsnorm(cx, g_norm1[l]), csh1, csc1)
        yl, yc = token_mixing(hc, hl, w_in[l], b_gate_m[l], a_log[l], dt_bias[l], w_conv[l],
                              g_head_m[l], g_head_d[l], w_out[l], not last)
        x = x + gt1 * yl
        hl2 = modulate(rmsnorm(x, g_norm2[l]), sh2, sc2)
        bsz, n, d = x.shape
        if last:
            x = x + gt2 * hier_moe(hl2.reshape(bsz * n, d), w_grp[l], b_grp[l], w_rtr[l], b_rtr[l],
                                   w1[l], w3[l], w2[l]).reshape(bsz, n, d)
        else:
            cx = cx + cgt1 * yc
            hc2 = modulate(rmsnorm(cx, g_norm2[l]), csh2, csc2)
            n_c = cx.shape[1]
            both = jnp.concatenate([hc2, hl2], axis=1)
            f = hier_moe(both.reshape(-1, d), w_grp[l], b_grp[l], w_rtr[l], b_rtr[l],
                         w1[l], w3[l], w2[l]).reshape(bsz, n_c + n, d)
            cx = cx + cgt2 * f[:, :n_c]
            x = x + gt2 * f[:, n_c:]
    return rmsnorm(x, g_final)
```

```python
import numpy as np
import contextlib
import ml_dtypes
import concourse.bass as bass
import concourse.mybir as mybir
from concourse.bass_utils import run_bass_kernel_spmd

F32 = mybir.dt.float32
BF16 = mybir.dt.bfloat16
I32 = mybir.dt.int32
ALU = mybir.AluOpType
AF = mybir.ActivationFunctionType
AX = mybir.AxisListType

SEM_LIMIT = 20000
N_DMA_SEMS = 24


class Tile:
    __slots__ = ("name", "h", "last_w", "readers", "excl")

    def __init__(self, name, h):
        self.name = name
        self.h = h
        self.last_w = None
        self.readers = []
        self.excl = False

    def __getitem__(self, k):
        return self.h[k]


class FW:
    def __init__(self, nc, stack):
        self.nc = nc
        self.stack = stack
        self.eng = {"pe": nc.tensor, "act": nc.scalar, "dve": nc.vector, "pool": nc.gpsimd, "sp": nc.sync}
        self.sem = {}
        self.cnt = {}
        self.pe_sems = []
        self.nsem = 0
        for e in self.eng:
            self._new_sem(e)
        self.seen = {e: {} for e in self.eng}
        self.dma_sems = [self._mk_sem("dma%d" % i) for i in range(2 * N_DMA_SEMS)]
        self.dma_cnt = [0] * (2 * N_DMA_SEMS)
        self.dma_i = [0, 0]
        self.n_tiles = 0
        self.out_tokens = []

    def _mk_sem(self, name):
        self.nsem += 1
        return self.stack.enter_context(self.nc.semaphore("%s_%d" % (name, self.nsem)))

    def _new_sem(self, e):
        self.sem[e] = self._mk_sem("s_" + e)
        self.cnt[e] = 0
        if e == "pe":
            self.pe_sems.append(self.sem[e])

    def sb(self, name, shape, dt=F32):
        self.n_tiles += 1
        h = self.nc.alloc_sbuf_tensor("%s_%d" % (name, self.n_tiles), list(shape), dt)
        return Tile(name, h)

    def ps(self, name, shape, dt=F32):
        self.n_tiles += 1
        h = self.stack.enter_context(self.nc.psum_tensor("%s_%d" % (name, self.n_tiles), list(shape), dt))
        t = Tile(name, h)
        t.excl = True
        return t

    def dram(self, name, shape, dt=F32, kind="Internal"):
        h = self.nc.dram_tensor(name, list(shape), dt, kind=kind)
        return Tile(name, h.ap())

    def _wait(self, e, tok):
        if tok is None:
            return
        sem, val = tok
        if e == "pe" and sem in self.pe_sems:
            return
        k = id(sem)
        if self.seen[e].get(k, 0) >= val:
            return
        self.seen[e][k] = val
        self.eng[e].wait_ge(sem, val)

    def _deps(self, e, reads, writes):
        for t in reads:
            self._wait(e, t.last_w)
            if t.excl:
                for r in t.readers:
                    self._wait(e, r)
        for t in writes:
            self._wait(e, t.last_w)
            for r in t.readers:
                self._wait(e, r)

    def _commit(self, tok, reads, writes):
        for t in reads:
            t.readers.append(tok)
            if len(t.readers) > 64:
                t.readers = t.readers[-64:]
        for t in writes:
            t.last_w = tok
            t.readers = []

    def op(self, e, fn, reads=(), writes=()):
        if self.cnt[e] >= SEM_LIMIT:
            self._new_sem(e)
        self._deps(e, reads, writes)
        ins = fn(self.eng[e])
        self.cnt[e] += 1
        ins.then_inc(self.sem[e], 1)
        tok = (self.sem[e], self.cnt[e])
        self._commit(tok, reads, writes)
        return tok

    def dma(self, q, out, in_, reads=(), writes=(), is_output=False, **kw):
        sw = 1 if q == "pool" else 0
        r = sw * N_DMA_SEMS + self.dma_i[sw] % N_DMA_SEMS
        self.dma_i[sw] += 1
        sem = self.dma_sems[r]
        if self.dma_cnt[r] > 0:
            self._wait(q, (sem, self.dma_cnt[r]))
        self._deps(q, reads, writes)
        ins = self.eng[q].dma_start(out=out, in_=in_, **kw)
        self.dma_cnt[r] += 16
        ins.then_inc(sem, 16)
        tok = (sem, self.dma_cnt[r])
        self._commit(tok, reads, writes)
        if is_output:
            self.out_tokens.append(tok)
        return tok

    def barrier(self):
        toks = [(self.sem[e], self.cnt[e]) for e in self.eng if self.cnt[e] > 0]
        toks += [(self.dma_sems[r], self.dma_cnt[r]) for r in range(2 * N_DMA_SEMS) if self.dma_cnt[r] > 0]
        for e in self.eng:
            for t in toks:
                if t[0] is self.sem[e]:
                    continue
                self._wait(e, t)

    def finish(self):
        for r in range(2 * N_DMA_SEMS):
            if self.dma_cnt[r] > 0:
                self._wait("sp", (self.dma_sems[r], self.dma_cnt[r]))
        for e in self.eng:
            if e != "sp" and self.cnt[e] > 0:
                self._wait("sp", (self.sem[e], self.cnt[e]))


D = 2048
NT2 = 16
NE = 32
DE = 768
EPS = 1e-6


def sbs(fw, stack, name, shape, dt=F32):
    fw.n_tiles += 1
    h = stack.enter_context(fw.nc.sbuf_tensor("%s_%d" % (name, fw.n_tiles), list(shape), dt))
    return Tile(name, h)


def build_l2(n_experts=NE, n_pass=2, stop=None, nt_d=NT2, cut=9):
    nc = bass.Bass("TRN2", target_bir_lowering=False)
    TOK = 2048
    with contextlib.ExitStack() as st:
        fw = FW(nc, st)
        def din(name, shape, dt=F32):
            return Tile(name, nc.dram_tensor(name, list(shape), dt, kind="ExternalInput").ap())
        x = din("x", [TOK, D])
        yT = din("yT", [D, TOK], BF16)
        c_l = din("c_l", [128, 16])
        w_ada2 = din("w_ada2", [D, 4 * D])
        b_ada2 = din("b_ada2", [1, 4 * D])
        gn2 = din("gn2", [D])
        gfin = din("gfin", [D])
        w_out = din("w_out", [D, D])
        wr = din("wr", [D, 36])
        br = din("br", [1, 36])
        if n_experts > 0:
            w1 = din("w1", [NE, D, DE])
            w3 = din("w3", [NE, D, DE])
            w2 = din("w2", [NE, DE, D])
        out = Tile("out", nc.dram_tensor("out", [TOK, D], F32, kind="ExternalOutput").ap())
        X1 = fw.dram("x1_scr", [TOK, D], F32)
        H2T = fw.dram("h2t_scr", [128, 16, TOK], BF16)

        ident = sbs(fw, st, "ident", [128, 128], F32)
        ones = sbs(fw, st, "ones", [128, 128], F32)
        epsT = sbs(fw, st, "eps", [128, 1], F32)
        GT2 = sbs(fw, st, "GT2", [128, D])
        GFIN = sbs(fw, st, "GFIN", [128, D])
        WT = sbs(fw, st, "WT", [128, NT2, NE])
        P = [fw.ps("P%d" % i, [128, 512], F32) for i in range(8)]

        fw.op("dve", lambda e: e.memset(ones[:], 1.0), writes=[ones])
        fw.op("dve", lambda e: e.memset(epsT[:], EPS), writes=[epsT])
        identd = din("identd", [128, 128])
        fw.dma("sp", ident[:], identd.h, reads=[identd], writes=[ident])
        fw.dma("sp", GFIN[:], gfin.h.partition_broadcast(128), reads=[gfin], writes=[GFIN])

        with contextlib.ExitStack() as sAD:
            GT1 = sbs(fw, sAD, "GT1", [128, D])
            G2 = sbs(fw, sAD, "G2", [128, D])
            SH2 = sbs(fw, sAD, "SH2", [128, D])
            with contextlib.ExitStack() as sA:
                csl = sbs(fw, sA, "csl", [128, 16])
                CB = sbs(fw, sA, "CB", [128, 16, 128])
                brow = sbs(fw, sA, "brow", [1, 4 * D])
                GN2 = sbs(fw, sA, "GN2", [128, D])
                Wst = [sbs(fw, sA, "Wst%d" % i, [128, 4096]) for i in range(2)]
                fw.dma("sp", csl[:], c_l.h, reads=[c_l], writes=[csl])
                fw.dma("sp", brow[:], b_ada2.h, reads=[b_ada2], writes=[brow])
                fw.dma("sp", GN2[:], gn2.h.partition_broadcast(128), reads=[gn2], writes=[GN2])
                fw.op("act", lambda e: e.activation(out=csl[:], in_=csl[:], func=AF.Silu), reads=[csl], writes=[csl])
                for k in range(16):
                    fw.op("dve", lambda e, k=k: e.tensor_scalar(out=CB[:, k, :], in0=ones[:], scalar1=csl[:, k:k + 1], scalar2=None, op0=ALU.mult),
                          reads=[ones, csl], writes=[CB])
                li = 0
                for half in range(2):
                    for k in range(16):
                        wt = Wst[li % 2]; li += 1
                        fw.dma("sp", wt[:], w_ada2.h[k * 128:(k + 1) * 128, half * 4096:(half + 1) * 4096], reads=[w_ada2], writes=[wt])
                        for j in range(8):
                            fw.op("pe", lambda e, j=j, k=k, wt=wt: e.matmul(P[j][:], lhsT=CB[:, k, :], rhs=wt[:, j * 512:(j + 1) * 512], start=(k == 0), stop=False),
                                  reads=[CB, wt], writes=[P[j]])
                    for j in range(8):
                        fw.op("pe", lambda e, j=j, half=half: e.matmul(P[j][:], lhsT=ones[0:1, :], rhs=brow[0:1, half * 4096 + j * 512: half * 4096 + (j + 1) * 512], start=False, stop=True),
                              reads=[ones, brow], writes=[P[j]])
                    for j in range(8):
                        cs = slice((j % 4) * 512, (j % 4 + 1) * 512)
                        if half == 0:
                            dst = GT1 if j < 4 else SH2
                            fw.op("act", lambda e, j=j, dst=dst, cs=cs: e.copy(out=dst[:, cs], in_=P[j][:]), reads=[P[j]], writes=[dst])
                        elif j < 4:
                            fw.op("dve", lambda e, j=j, cs=cs: e.scalar_tensor_tensor(out=G2[:, cs], in0=P[j][:], scalar=1.0, in1=GN2[:, cs], op0=ALU.add, op1=ALU.mult),
                                  reads=[P[j], GN2], writes=[G2])
                        else:
                            fw.op("act", lambda e, j=j, cs=cs: e.copy(out=GT2[:, cs], in_=P[j][:]), reads=[P[j]], writes=[GT2])
                fw.barrier()
                if stop == "A":
                    fw.dma("sp", out.h[0:128, :], GT1[:], reads=[GT1], writes=[out], is_output=True)
                    fw.dma("sp", out.h[128:256, :], G2[:], reads=[G2], writes=[out], is_output=True)
                    fw.dma("sp", out.h[256:384, :], SH2[:], reads=[SH2], writes=[out], is_output=True)
                    fw.dma("sp", out.h[384:512, :], GT2[:], reads=[GT2], writes=[out], is_output=True)
                    fw.finish()
                    return nc
            with contextlib.ExitStack() as sD:
                WO = sbs(fw, sD, "WO", [128, 16, D], BF16)
                WR = sbs(fw, sD, "WR", [128, 16, 36])
                brr = sbs(fw, sD, "brr", [1, 36])
                xt = [sbs(fw, sD, "xt%d" % i, [128, D]) for i in range(2)]
                yt = [sbs(fw, sD, "yt%d" % i, [128, 16, 128], BF16) for i in range(2)]
                tmp = [sbs(fw, sD, "tmp%d" % i, [128, D]) for i in range(2)]
                hTf = sbs(fw, sD, "hTf", [128, 16, 128])
                hTb = [sbs(fw, sD, "hTb%d" % i, [128, 16, 128], BF16) for i in range(2)]
                sm = {n: sbs(fw, sD, n, [128, w]) for n, w in
                      [("ss", 1), ("rstd", 1), ("lg", 36), ("gmax", 1), ("ngmax", 1), ("eg", 4), ("sumg", 1), ("pgrp", 1), ("oh", 4), ("pen", 4),
                       ("lm", 32), ("top8", 8), ("sel", 32), ("nm1", 1), ("ew", 32), ("den", 1), ("fac", 1)]}
                fw.dma("pool", WO[:], w_out.h.rearrange("(k p) n -> p k n", p=128), reads=[w_out], writes=[WO])
                fw.dma("sp", WR[:], wr.h.rearrange("(k p) n -> p k n", p=128), reads=[wr], writes=[WR])
                fw.dma("sp", brr[:], br.h, reads=[br], writes=[brr])
                yTv = yT.h.rearrange("(k p) t -> p k t", p=128)
                for t in range(nt_d):
                    xb, yb, t1, t2 = xt[t % 2], yt[t % 2], tmp[0], tmp[1]
                    ts = slice(t * 128, (t + 1) * 128)
                    fw.dma("sp", xb[:], x.h[ts, :], reads=[x], writes=[xb])
                    fw.dma("sp", yb[:], yTv[:, :, ts], reads=[yT], writes=[yb])
                    for nb in range(4):
                        for k in range(16):
                            fw.op("pe", lambda e, nb=nb, k=k, yb=yb: e.matmul(P[nb][:], lhsT=yb[:, k, :], rhs=WO[:, k, nb * 512:(nb + 1) * 512], start=(k == 0), stop=(k == 15)),
                                  reads=[yb, WO], writes=[P[nb]])
                    for nb in range(4):
                        cs = slice(nb * 512, (nb + 1) * 512)
                        fw.op("dve", lambda e, nb=nb, cs=cs: e.tensor_tensor(out=t1[:, cs], in0=P[nb][:], in1=GT1[:, cs], op=ALU.mult), reads=[P[nb], GT1], writes=[t1])
                    fw.op("dve", lambda e, xb=xb: e.tensor_tensor(out=xb[:], in0=t1[:], in1=xb[:], op=ALU.add), reads=[t1, xb], writes=[xb])
                    fw.dma("sp", X1.h[ts, :], xb[:], reads=[xb], writes=[X1])
                    if cut <= 1: continue
                    fw.op("act", lambda e, xb=xb: e.activation(out=t1[:], in_=xb[:], func=AF.Square, accum_out=sm["ss"][:]), reads=[xb], writes=[t1, sm["ss"]])
                    fw.op("act", lambda e: e.activation(out=sm["rstd"][:], in_=sm["ss"][:], func=AF.Sqrt, bias=epsT[:], scale=1.0 / D), reads=[sm["ss"], epsT], writes=[sm["rstd"]])
                    fw.op("dve", lambda e: e.reciprocal(out=sm["rstd"][:], in_=sm["rstd"][:]), reads=[sm["rstd"]], writes=[sm["rstd"]])
                    fw.op("dve", lambda e, xb=xb: e.scalar_tensor_tensor(out=t2[:], in0=xb[:], scalar=sm["rstd"][:], in1=G2[:], op0=ALU.mult, op1=ALU.mult),
                          reads=[xb, sm["rstd"], G2], writes=[t2])
                    fw.op("dve", lambda e: e.tensor_tensor(out=t2[:], in0=t2[:], in1=SH2[:], op=ALU.add), reads=[t2, SH2], writes=[t2])
                    if cut <= 2: continue
                    hb = hTb[t % 2]
                    for k in range(16):
                        pb = P[4 + k // 4]
                        fw.op("pe", lambda e, k=k, pb=pb: e.transpose(out=pb[:, (k % 4) * 128:(k % 4 + 1) * 128], in_=t2[:, k * 128:(k + 1) * 128], identity=ident[:]),
                              reads=[t2, ident], writes=[pb])
                    for q in range(4):
                        pb = P[4 + q]
                        fw.op("act", lambda e, q=q, pb=pb: e.copy(out=hTf[:, q * 4:(q + 1) * 4, :], in_=pb[:].rearrange("p (a b) -> p a b", a=4)), reads=[pb], writes=[hTf])
                        fw.op("dve", lambda e, q=q, hb=hb: e.tensor_copy(out=hb[:, q * 4:(q + 1) * 4, :], in_=hTf[:, q * 4:(q + 1) * 4, :]), reads=[hTf], writes=[hb])
                    fw.dma("sp", H2T.h[:, :, ts], hb[:], reads=[hb], writes=[H2T])
                    if cut <= 3: continue
                    for k in range(16):
                        fw.op("pe", lambda e, k=k: e.matmul(P[0][:, 0:36], lhsT=hTf[:, k, :], rhs=WR[:, k, :], start=(k == 0), stop=False), reads=[hTf, WR], writes=[P[0]])
                    fw.op("pe", lambda e: e.matmul(P[0][:, 0:36], lhsT=ones[0:1, :], rhs=brr[0:1, :], start=False, stop=True), reads=[ones, brr], writes=[P[0]])
                    S = sm
                    if cut <= 4: continue
                    fw.op("act", lambda e: e.copy(out=S["lg"][:], in_=P[0][:, 0:36]), reads=[P[0]], writes=[S["lg"]])
                    fw.op("dve", lambda e: e.reduce_max(out=S["gmax"][:], in_=S["lg"][:, 0:4], axis=AX.X), reads=[S["lg"]], writes=[S["gmax"]])
                    fw.op("dve", lambda e: e.tensor_scalar(out=S["ngmax"][:], in0=S["gmax"][:], scalar1=-1.0, scalar2=None, op0=ALU.mult), reads=[S["gmax"]], writes=[S["ngmax"]])
                    fw.op("act", lambda e: e.activation(out=S["eg"][:], in_=S["lg"][:, 0:4], func=AF.Exp, bias=S["ngmax"][:], scale=1.0, accum_out=S["sumg"][:]),
                          reads=[S["lg"], S["ngmax"]], writes=[S["eg"], S["sumg"]])
                    fw.op("dve", lambda e: e.reciprocal(out=S["pgrp"][:], in_=S["sumg"][:]), reads=[S["sumg"]], writes=[S["pgrp"]])
                    fw.op("dve", lambda e: e.tensor_scalar(out=S["oh"][:], in0=S["lg"][:, 0:4], scalar1=S["gmax"][:], scalar2=None, op0=ALU.is_ge), reads=[S["lg"], S["gmax"]], writes=[S["oh"]])
                    fw.op("dve", lambda e: e.tensor_scalar(out=S["pen"][:], in0=S["oh"][:], scalar1=-1.0, scalar2=1e30, op0=ALU.add, op1=ALU.mult), reads=[S["oh"]], writes=[S["pen"]])
                    for g in range(4):
                        fw.op("dve", lambda e, g=g: e.tensor_scalar(out=S["lm"][:, g * 8:(g + 1) * 8], in0=S["lg"][:, 4 + g * 8:4 + (g + 1) * 8], scalar1=S["pen"][:, g:g + 1], scalar2=None, op0=ALU.add),
                              reads=[S["lg"], S["pen"]], writes=[S["lm"]])
                    fw.op("dve", lambda e: e.max(out=S["top8"][:], in_=S["lm"][:]), reads=[S["lm"]], writes=[S["top8"]])
                    fw.op("dve", lambda e: e.tensor_scalar(out=S["sel"][:], in0=S["lm"][:], scalar1=S["top8"][:, 1:2], scalar2=None, op0=ALU.is_ge), reads=[S["lm"], S["top8"]], writes=[S["sel"]])
                    fw.op("dve", lambda e: e.tensor_scalar(out=S["nm1"][:], in0=S["top8"][:, 0:1], scalar1=-1.0, scalar2=None, op0=ALU.mult), reads=[S["top8"]], writes=[S["nm1"]])
                    fw.op("act", lambda e: e.activation(out=S["ew"][:], in_=S["lm"][:], func=AF.Exp, bias=S["nm1"][:], scale=1.0), reads=[S["lm"], S["nm1"]], writes=[S["ew"]])
                    fw.op("dve", lambda e: e.tensor_tensor(out=S["ew"][:], in0=S["ew"][:], in1=S["sel"][:], op=ALU.mult), reads=[S["ew"], S["sel"]], writes=[S["ew"]])
                    fw.op("dve", lambda e: e.reduce_sum(out=S["den"][:], in_=S["ew"][:], axis=AX.X), reads=[S["ew"]], writes=[S["den"]])
                    fw.op("dve", lambda e: e.reciprocal(out=S["fac"][:], in_=S["den"][:]), reads=[S["den"]], writes=[S["fac"]])
                    fw.op("dve", lambda e: e.tensor_tensor(out=S["fac"][:], in0=S["fac"][:], in1=S["pgrp"][:], op=ALU.mult), reads=[S["fac"], S["pgrp"]], writes=[S["fac"]])
                    fw.op("dve", lambda e, t=t: e.tensor_scalar(out=WT[:, t, :], in0=S["ew"][:], scalar1=S["fac"][:], scalar2=None, op0=ALU.mult), reads=[S["ew"], S["fac"]], writes=[WT])
                fw.barrier()
        TP = TOK // n_pass
        NTP = TP // 128
        NG = TP // 512
        with contextlib.ExitStack() as sM:
            ACC = sbs(fw, sM, "ACC", [128, NTP, D])
            HP = sbs(fw, sM, "HP", [128, 16, TP], BF16)
            w1c = [sbs(fw, sM, "w1c%d" % i, [128, 16, 128], BF16) for i in range(2)]
            w3c = [sbs(fw, sM, "w3c%d" % i, [128, 16, 128], BF16) for i in range(2)]
            w2c = [sbs(fw, sM, "w2c%d" % i, [128, D], BF16) for i in range(8)]
            G = sbs(fw, sM, "G", [128, 6, TP], BF16)
            sl = [sbs(fw, sM, "sl%d" % i, [128, 512]) for i in range(2)]
            x1t = [sbs(fw, sM, "x1t%d" % i, [128, D]) for i in range(2)]
            ss2 = sbs(fw, sM, "ss2", [128, 1])
            rs2 = sbs(fw, sM, "rs2", [128, 1])
            ci = 0; w2i = 0; si = 0
            for p in range(n_pass):
                fw.dma("sp", HP[:], H2T.h[:, :, p * TP:(p + 1) * TP], reads=[H2T], writes=[HP])
                for ex in range(n_experts):
                    w1v = w1.h[ex].rearrange("(k p) n -> p k n", p=128)
                    w3v = w3.h[ex].rearrange("(k p) n -> p k n", p=128)
                    w2v = w2.h[ex].rearrange("(k p) n -> p k n", p=128)
                    w2l = []
                    for kc in range(6):
                        wb = w2c[w2i % 8]; w2i += 1
                        fw.dma("pool", wb[:], w2v[:, kc, :], reads=[w2], writes=[wb])
                        w2l.append(wb)
                    for cc in range(6):
                        a1, a3 = w1c[ci % 2], w3c[ci % 2]; ci += 1
                        fw.dma("pool", a1[:], w1v[:, :, cc * 128:(cc + 1) * 128], reads=[w1], writes=[a1])
                        fw.dma("pool", a3[:], w3v[:, :, cc * 128:(cc + 1) * 128], reads=[w3], writes=[a3])
                        for tg in range(NG):
                            pu1, pu3 = P[(2 * tg) % 4], P[(2 * tg + 1) % 4]
                            gs = slice(tg * 512, (tg + 1) * 512)
                            for k in range(16):
                                fw.op("pe", lambda e, k=k, a1=a1, pu1=pu1, gs=gs: e.matmul(pu1[:], lhsT=a1[:, k, :], rhs=HP[:, k, gs], start=(k == 0), stop=(k == 15)), reads=[a1, HP], writes=[pu1])
                            for k in range(16):
                                fw.op("pe", lambda e, k=k, a3=a3, pu3=pu3, gs=gs: e.matmul(pu3[:], lhsT=a3[:, k, :], rhs=HP[:, k, gs], start=(k == 0), stop=(k == 15)), reads=[a3, HP], writes=[pu3])
                            sb_ = sl[si % 2]; si += 1
                            fw.op("act", lambda e, sb_=sb_, pu1=pu1: e.activation(out=sb_[:], in_=pu1[:], func=AF.Silu), reads=[pu1], writes=[sb_])
                            fw.op("dve", lambda e, sb_=sb_, pu3=pu3, cc=cc, gs=gs: e.tensor_tensor(out=G[:, cc, gs], in0=sb_[:], in1=pu3[:], op=ALU.mult), reads=[sb_, pu3], writes=[G])
                    for tl in range(NTP):
                        tglob = p * NTP + tl
                        for nb in range(4):
                            pd = P[4 + (tl * 4 + nb) % 4]
                            cs = slice(nb * 512, (nb + 1) * 512)
                            for kc in range(6):
                                fw.op("pe", lambda e, kc=kc, pd=pd, tl=tl, cs=cs: e.matmul(pd[:], lhsT=G[:, kc, tl * 128:(tl + 1) * 128], rhs=w2l[kc][:, cs], start=(kc == 0), stop=(kc == 5)),
                                      reads=[G, w2l[kc]], writes=[pd])
                            if ex == 0:
                                fw.op("dve", lambda e, pd=pd, tl=tl, cs=cs, tglob=tglob: e.tensor_scalar(out=ACC[:, tl, cs], in0=pd[:], scalar1=WT[:, tglob, 0:1], scalar2=None, op0=ALU.mult),
                                      reads=[pd, WT], writes=[ACC])
                            else:
                                fw.op("dve", lambda e, pd=pd, tl=tl, cs=cs, tglob=tglob, ex=ex: e.scalar_tensor_tensor(out=ACC[:, tl, cs], in0=pd[:], scalar=WT[:, tglob, ex:ex + 1], in1=ACC[:, tl, cs], op0=ALU.mult, op1=ALU.add),
                                      reads=[pd, WT, ACC], writes=[ACC])
                for tl in range(NTP):
                    tglob = p * NTP + tl
                    ts = slice(tglob * 128, (tglob + 1) * 128)
                    xb = x1t[tl % 2]
                    fw.dma("sp", xb[:], X1.h[ts, :], reads=[X1], writes=[xb])
                    fw.op("dve", lambda e, tl=tl: e.tensor_tensor(out=ACC[:, tl, :], in0=ACC[:, tl, :], in1=GT2[:], op=ALU.mult), reads=[ACC, GT2], writes=[ACC])
                    fw.op("dve", lambda e, tl=tl, xb=xb: e.tensor_tensor(out=xb[:], in0=ACC[:, tl, :], in1=xb[:], op=ALU.add), reads=[ACC, xb], writes=[xb])
                    fw.op("act", lambda e, tl=tl, xb=xb: e.activation(out=ACC[:, tl, :], in_=xb[:], func=AF.Square, accum_out=ss2[:]), reads=[xb], writes=[ACC, ss2])
                    fw.op("act", lambda e: e.activation(out=rs2[:], in_=ss2[:], func=AF.Sqrt, bias=epsT[:], scale=1.0 / D), reads=[ss2, epsT], writes=[rs2])
                    fw.op("dve", lambda e: e.reciprocal(out=rs2[:], in_=rs2[:]), reads=[rs2], writes=[rs2])
                    fw.op("dve", lambda e, xb=xb: e.scalar_tensor_tensor(out=xb[:], in0=xb[:], scalar=rs2[:], in1=GFIN[:], op0=ALU.mult, op1=ALU.mult), reads=[xb, rs2, GFIN], writes=[xb])
                    fw.dma("sp", out.h[ts, :], xb[:], reads=[xb], writes=[out], is_output=True)
            fw.finish()
    return nc


D = 2048
EPS = 1e-6
NEG = -30000.0
LV = [1, 2, 4, 8, 16, 32, 64]


def l1_consts():
    p = np.arange(128)[:, None]; f = np.arange(128)[None, :]
    tri0 = (p <= f).astype(np.float32)
    ni0 = np.where(p <= f, 0.0, NEG).astype(np.float32)
    ns0 = np.where(p < f, 0.0, NEG).astype(np.float32)
    mats = [tri0, tri0.T, ni0, ni0.T, ns0, ns0.T]
    for s in LV:
        m = ((p // (2 * s) == f // (2 * s)) & (p % (2 * s) >= s) & (f % (2 * s) < s)).astype(np.float32)
        mats += [m, m.T]
    mats.append(np.eye(128, dtype=np.float32))
    return np.ascontiguousarray(np.stack(mats, axis=1))


def build_l1(NBL=64, stop=None):
    nc = bass.Bass("TRN2", target_bir_lowering=False)
    NB = NBL + 2
    with contextlib.ExitStack() as st:
        fw = FW(nc, st)
        def din(name, shape, dt=F32):
            return Tile(name, nc.dram_tensor(name, list(shape), dt, kind="ExternalInput").ap())
        x = din("x", [NBL * 128, D]); ctx = din("ctx", [256, D])
        c_l = din("c_l", [128, 16]); cc_l = din("cc_l", [128, 16])
        w_ada1 = din("w_ada1", [D, 2 * D]); b_ada1 = din("b_ada1", [1, 2 * D])
        gn1 = din("gn1", [D])
        wF = din("wF", [D, 1024]); wT = din("wT", [D, 908])
        wconv = din("wconv", [128, 6, 5])
        gbias = din("gbias", [4]); alog = din("alog", [4]); dtb = din("dtb", [4])
        ghm = din("ghm", [256]); ghd = din("ghd", [256])
        constd = din("constd", [128, 21, 128])
        y = Tile("y", nc.dram_tensor("y", [NBL * 128, 512], BF16, kind="ExternalOutput").ap())
        SM_FM = fw.dram("sm_fm", [NB, 128, 2, 128])
        SM_TM = fw.dram("sm_tm", [NB, 128, 385])
        SG_FM = fw.dram("sg_fm", [NB, 128, 4, 128])
        SG_TM = fw.dram("sg_tm", [NB, 128, 4, 128])
        SMALL = fw.dram("small", [NB, 128, 12])
        OG = fw.dram("og", [NBL, 128, 512])
        H0 = fw.dram("h0", [NBL, 128, 512])

        CONST = sbs(fw, st, "CONST", [128, 21, 128])
        fw.dma("sp", CONST[:], constd.h, reads=[constd], writes=[CONST])
        TRI = [CONST[:, 0, :], CONST[:, 1, :]]
        NI = [CONST[:, 2, :], CONST[:, 3, :]]
        NS = [CONST[:, 4, :], CONST[:, 5, :]]
        def MK(d, li): return CONST[:, 6 + 2 * li + d, :]
        def MKT(d, li): return CONST[:, 6 + 2 * li + (1 - d), :]
        ident = CONST[:, 20, :]
        ones = sbs(fw, st, "ones", [128, 128])
        epsT = sbs(fw, st, "eps", [128, 1])
        identb = sbs(fw, st, "identb", [128, 128], BF16)
        fw.op("dve", lambda e: e.memset(ones[:], 1.0), writes=[ones])
        fw.op("dve", lambda e: e.memset(epsT[:], EPS), writes=[epsT])
        fw.op("dve", lambda e: e.tensor_copy(out=identb[:], in_=ident), reads=[CONST], writes=[identb])
        P = [fw.ps("P%d" % i, [128, 512], F32) for i in range(8)]
        pbank = [0]
        def nb_():
            pbank[0] += 1
            return P[pbank[0] % 8]

        def TT(e, out, a, b, op, R, W): return fw.op(e, lambda en: en.tensor_tensor(out=out, in0=a, in1=b, op=op), reads=R, writes=W)
        def TS(e, out, a, s1, op0, R, W, s2=None, op1=None):
            if op1 is None:
                return fw.op(e, lambda en: en.tensor_scalar(out=out, in0=a, scalar1=s1, scalar2=None, op0=op0), reads=R, writes=W)
            return fw.op(e, lambda en: en.tensor_scalar(out=out, in0=a, scalar1=s1, scalar2=s2, op0=op0, op1=op1), reads=R, writes=W)
        def STT(e, out, a, s, b, op0, op1, R, W): return fw.op(e, lambda en: en.scalar_tensor_tensor(out=out, in0=a, scalar=s, in1=b, op0=op0, op1=op1), reads=R, writes=W)
        def ACT(out, in_, func, R, W, bias=None, scale=None, accum=None):
            kw = {}
            if bias is not None: kw["bias"] = bias
            if scale is not None: kw["scale"] = scale
            if accum is not None: kw["accum_out"] = accum
            return fw.op("act", lambda en: en.activation(out=out, in_=in_, func=func, **kw), reads=R, writes=W)
        def MM(out, lhsT, rhs, start, stop, R, W): return fw.op("pe", lambda en: en.matmul(out, lhsT=lhsT, rhs=rhs, start=start, stop=stop), reads=R, writes=W)
        def TR(out, in_, idn, R, W): return fw.op("pe", lambda en: en.transpose(out=out, in_=in_, identity=idn), reads=R, writes=W)
        def CP(e, out, in_, R, W):
            if e == "act":
                return fw.op("act", lambda en: en.copy(out=out, in_=in_), reads=R, writes=W)
            return fw.op(e, lambda en: en.tensor_copy(out=out, in_=in_), reads=R, writes=W)

        with contextlib.ExitStack() as sAB:
            G1 = sbs(fw, sAB, "G1", [128, D]); SH1 = sbs(fw, sAB, "SH1", [128, D])
            CG1 = sbs(fw, sAB, "CG1", [128, D]); CSH1 = sbs(fw, sAB, "CSH1", [128, D])
            with contextlib.ExitStack() as sA:
                GN1 = sbs(fw, sA, "GN1", [128, D])
                brow = sbs(fw, sA, "brow", [1, 2 * D])
                Wst = [sbs(fw, sA, "Wst%d" % i, [128, 4096]) for i in range(2)]
                fw.dma("sp", brow[:], b_ada1.h, reads=[b_ada1], writes=[brow])
                fw.dma("sp", GN1[:], gn1.h.partition_broadcast(128), reads=[gn1], writes=[GN1])
                li = 0
                for (cv, Gd, SHd) in ((c_l, G1, SH1), (cc_l, CG1, CSH1)):
                    csl = sbs(fw, sA, "csl", [128, 16]); CB = sbs(fw, sA, "CB", [128, 16, 128])
                    fw.dma("sp", csl[:], cv.h, reads=[cv], writes=[csl])
                    ACT(csl[:], csl[:], AF.Silu, [csl], [csl])
                    for k in range(16):
                        TS("dve", CB[:, k, :], ones[:], csl[:, k:k + 1], ALU.mult, [ones, csl], [CB])
                    for k in range(16):
                        wt = Wst[li % 2]; li += 1
                        fw.dma("sp", wt[:], w_ada1.h[k * 128:(k + 1) * 128, :], reads=[w_ada1], writes=[wt])
                        for j in range(8):
                            MM(P[j][:], CB[:, k, :], wt[:, j * 512:(j + 1) * 512], k == 0, False, [CB, wt], [P[j]])
                    for j in range(8):
                        MM(P[j][:], ones[0:1, :], brow[0:1, j * 512:(j + 1) * 512], False, True, [ones, brow], [P[j]])
                    for j in range(8):
                        cs = slice((j % 4) * 512, (j % 4 + 1) * 512)
                        if j < 4:
                            CP("act", SHd[:, cs], P[j][:], [P[j]], [SHd])
                        else:
                            STT("dve", Gd[:, cs], P[j][:], 1.0, GN1[:, cs], ALU.add, ALU.mult, [P[j], GN1], [Gd])
                fw.barrier()
            if stop == "A":
                for i, tl in enumerate((G1, SH1, CG1, CSH1)):
                    fw.dma("sp", y.h[i * 128:(i + 1) * 128, :].bitcast(F32) if False else H0.h[0, :, :], tl[:, 0:512], reads=[tl], writes=[H0])
                fw.finish(); return nc
            with contextlib.ExitStack() as sB:
                WFs = sbs(fw, sB, "WFs", [128, 16, 1024], BF16)
                WTs = sbs(fw, sB, "WTs", [128, 16, 908], BF16)
                WC = sbs(fw, sB, "WC", [128, 6, 5])
                fw.dma("pool", WFs[:], wF.h.rearrange("(k p) n -> p k n", p=128), reads=[wF], writes=[WFs])
                fw.dma("pool", WTs[:], wT.h.rearrange("(k p) n -> p k n", p=128), reads=[wT], writes=[WTs])
                fw.dma("sp", WC[:], wconv.h, reads=[wconv], writes=[WC])
                xt = [sbs(fw, sB, "xt%d" % i, [128, D]) for i in range(2)]
                junk = sbs(fw, sB, "junk", [128, D])
                hb = sbs(fw, sB, "hb", [128, D], BF16)
                hT = sbs(fw, sB, "hT", [128, 16, 128], BF16)
                ss = sbs(fw, sB, "ss", [128, 1]); rstd = sbs(fw, sB, "rstd", [128, 1])
                fmm = [sbs(fw, sB, "fmm%d" % i, [128, 2, 128]) for i in range(2)]
                tmm = [sbs(fw, sB, "tmm%d" % i, [128, 385]) for i in range(2)]
                ogt = [sbs(fw, sB, "ogt%d" % i, [128, 512]) for i in range(2)]
                smt = [sbs(fw, sB, "smt%d" % i, [128, 12]) for i in range(2)]
                for t_ in tmm:
                    fw.op("dve", lambda e, t_=t_: e.memset(t_[:, 384:385], 1.0), writes=[t_])
                pc = sbs(fw, sB, "pc", [128, 6, 256]); cv_ = sbs(fw, sB, "cv", [128, 6, 256])
                sq = sbs(fw, sB, "sq", [128, 4, 256]); rn = sbs(fw, sB, "rn", [128, 4, 256])
                gfm = [sbs(fw, sB, "gfm%d" % i, [128, 4, 128]) for i in range(2)]
                gtm = [sbs(fw, sB, "gtm%d" % i, [128, 4, 128]) for i in range(2)]
                Pb = [P[i][:].bitcast(BF16) for i in range(8)]
                cnt = [0]

                def project(src, row0, blk, Gm, SHm, col0, is_lat):
                    i = cnt[0]; cnt[0] += 1
                    xb = xt[i % 2]
                    fw.dma("sp", xb[:], src.h[row0:row0 + 128, :], reads=[src], writes=[xb])
                    ACT(junk[:], xb[:], AF.Square, [xb], [junk, ss], accum=ss[:])
                    ACT(rstd[:], ss[:], AF.Sqrt, [ss, epsT], [rstd], bias=epsT[:], scale=1.0 / D)
                    fw.op("dve", lambda e: e.reciprocal(out=rstd[:], in_=rstd[:]), reads=[rstd], writes=[rstd])
                    STT("dve", junk[:], xb[:], rstd[:], Gm[:], ALU.mult, ALU.mult, [xb, rstd, Gm], [junk])
                    TT("dve", hb[:], junk[:], SHm[:], ALU.add, [junk, SHm], [hb])
                    pa, pb2 = (0, 1)
                    for k in range(16):
                        bk = P[k // 8]
                        TR(Pb[k // 8][:, (k % 8) * 128:(k % 8 + 1) * 128], hb[:, k * 128:(k + 1) * 128], identb[:], [hb, identb], [bk])
                    for q in range(2):
                        CP("act" if q == 0 else "dve", hT[:, q * 8:(q + 1) * 8, :], Pb[q].rearrange("p (a b) -> p a b", a=8), [P[q]], [hT])
                    for c in range(8):
                        bk = P[2 + c // 4]
                        for k in range(16):
                            MM(bk[:, (c % 4) * 128:(c % 4 + 1) * 128], WFs[:, k, c * 128:(c + 1) * 128], hT[:, k, :], k == 0, k == 15, [WFs, hT], [bk])
                    for k in range(16):
                        MM(P[4][:], hT[:, k, :], WTs[:, k, 0:512], k == 0, k == 15, [hT, WTs], [P[4]])
                    for k in range(16):
                        MM(P[5][:, 0:396], hT[:, k, :], WTs[:, k, 512:908], k == 0, k == 15, [hT, WTs], [P[5]])
                    fm = fmm[i % 2]; tm = tmm[i % 2]; og = ogt[i % 2]; sm_ = smt[i % 2]
                    TS("dve", fm[:, 0, :], P[2][:, 0:128], 128.0 ** -0.5, ALU.mult, [P[2]], [fm])
                    CP("dve", fm[:, 1, :], P[2][:, 128:256], [P[2]], [fm])
                    fw.dma("sp", SM_FM.h[blk], fm[:], reads=[fm], writes=[SM_FM])
                    CP("act", pc[:, 0:2, col0:col0 + 128], P[2][:, 256:512].rearrange("p (a b) -> p a b", a=2), [P[2]], [pc])
                    CP("act", pc[:, 2:6, col0:col0 + 128], P[3][:].rearrange("p (a b) -> p a b", a=4), [P[3]], [pc])
                    CP("act", tm[:, 0:384], P[4][:, 0:384], [P[4]], [tm])
                    fw.dma("sp", SM_TM.h[blk], tm[:], reads=[tm], writes=[SM_TM])
                    CP("dve", sm_[:], P[5][:, 384:396], [P[5]], [sm_])
                    fw.dma("sp", SMALL.h[blk], sm_[:], reads=[sm_], writes=[SMALL])
                    if is_lat:
                        CP("act", og[:, 0:256], P[5][:, 0:256], [P[5]], [og])
                        CP("dve", og[:, 256:384], P[4][:, 384:512], [P[4]], [og])
                        CP("dve", og[:, 384:512], P[5][:, 256:384], [P[5]], [og])
                        fw.dma("sp", OG.h[blk], og[:], reads=[og], writes=[OG])

                def gdn_post(blks, W, R):
                    nr = W // R
                    for c in range(6):
                        src = pc[:, c, 0:W]; dst = cv_[:, c, 0:W]
                        TS("dve", dst, src, WC[:, c, 2:3], ALU.mult, [pc, WC], [cv_])
                        s3 = src.rearrange("p (r w) -> p r w", w=R); d3 = dst.rearrange("p (r w) -> p r w", w=R)
                        for j in (0, 1, 3, 4):
                            dd = j - 2
                            lo_o, hi_o = max(0, -dd), R - max(0, dd)
                            STT("dve", d3[:, :, lo_o:hi_o], s3[:, :, lo_o + dd:hi_o + dd], WC[:, c, j:j + 1], d3[:, :, lo_o:hi_o], ALU.mult, ALU.add, [pc, WC, cv_], [cv_])
                    ACT(cv_[:, :, 0:W], cv_[:, :, 0:W], AF.Silu, [cv_], [cv_])
                    TT("dve", sq[:, :, 0:W], cv_[:, 0:4, 0:W], cv_[:, 0:4, 0:W], ALU.mult, [cv_], [sq])
                    for a in range(4):
                        for h_ in range(W // 128):
                            bk = P[6 + (a * (W // 128) + h_) // 4]
                            off = ((a * (W // 128) + h_) % 4) * 128
                            MM(bk[:, off:off + 128], ones[:], sq[:, a, h_ * 128:(h_ + 1) * 128], True, True, [ones, sq], [bk])
                    nt = 4 * (W // 128)
                    for a in range(4):
                        for h_ in range(W // 128):
                            idx = a * (W // 128) + h_
                            bk = P[6 + idx // 4]; off = (idx % 4) * 128
                            ACT(rn[:, a, h_ * 128:(h_ + 1) * 128], bk[:, off:off + 128], AF.Sqrt, [bk, epsT], [rn], bias=epsT[:], scale=1.0)
                    fw.op("dve", lambda e: e.reciprocal(out=rn[:, :, 0:W], in_=rn[:, :, 0:W]), reads=[rn], writes=[rn])
                    STT("dve", cv_[:, 0:2, 0:W], cv_[:, 0:2, 0:W], 128.0 ** -0.5, rn[:, 0:2, 0:W], ALU.mult, ALU.mult, [cv_, rn], [cv_])
                    TT("dve", cv_[:, 2:4, 0:W], cv_[:, 2:4, 0:W], rn[:, 2:4, 0:W], ALU.mult, [cv_, rn], [cv_])
                    for bi, blk in enumerate(blks):
                        i = cnt[0]; cnt[0] += 1
                        gf = gfm[i % 2]; gt = gtm[i % 2]
                        cs = slice(bi * 128, (bi + 1) * 128)
                        CP("act", gf[:], cv_[:, 0:4, cs], [cv_], [gf])
                        fw.dma("sp", SG_FM.h[blk], gf[:], reads=[gf], writes=[SG_FM])
                        bk = nb_()
                        for a in range(4):
                            TR(bk[:, a * 128:(a + 1) * 128], cv_[:, 2 + a, cs], ident, [cv_, CONST], [bk])
                        CP("act", gt[:], bk[:].rearrange("p (a b) -> p a b", a=4), [bk], [gt])
                        fw.dma("sp", SG_TM.h[blk], gt[:], reads=[gt], writes=[SG_TM])

                project(ctx, 0, NBL, CG1, CSH1, 0, False)
                project(ctx, 128, NBL + 1, CG1, CSH1, 128, False)
                gdn_post([NBL, NBL + 1], 256, 256)
                for t in range(NBL):
                    project(x, t * 128, t, G1, SH1, 0, True)
                    gdn_post([t], 128, 64)
                fw.barrier()
        if stop == "B":
            fw.finish(); return nc, dict(SM_FM=SM_FM, SM_TM=SM_TM, SG_FM=SG_FM, SG_TM=SG_TM, SMALL=SMALL, OG=OG)
        build_l1_scan(nc, fw, st, locals())
        fw.finish()
    return nc


def build_l1_scan(nc, fw, st, L):
    NBL, NB = L["NBL"], L["NB"]
    TT, TS, STT, ACT, MM, TR, CP, nb_ = L["TT"], L["TS"], L["STT"], L["ACT"], L["MM"], L["TR"], L["CP"], L["nb_"]
    TRI, NI, NS, MK, MKT, ident, ones, epsT, CONST = L["TRI"], L["NI"], L["NS"], L["MK"], L["MKT"], L["ident"], L["ones"], L["epsT"], L["CONST"]
    SM_FM, SM_TM, SG_FM, SG_TM, SMALL, OG, H0, y = L["SM_FM"], L["SM_TM"], L["SG_FM"], L["SG_TM"], L["SMALL"], L["OG"], L["H0"], L["y"]
    gbias, alog, dtb, ghm, ghd = L["gbias"], L["alog"], L["dtb"], L["ghm"], L["ghd"]
    NST = [NS[1], NS[0]]
    with contextlib.ExitStack() as sC:
        def S_(name, shape, dt=F32): return sbs(fw, sC, name, shape, dt)
        SMA = S_("SMA", [128, NB, 12])
        for blk in range(NB):
            fw.dma("sp", SMA[:, blk, :], SMALL.h[blk], reads=[SMALL], writes=[SMA])
        GB = S_("GB", [128, 4]); AL = S_("AL", [128, 4]); DT = S_("DT", [128, 4])
        GHM = S_("GHM", [128, 256]); GHD = S_("GHD", [128, 256])
        for tl, src in ((GB, gbias), (AL, alog), (DT, dtb), (GHM, ghm), (GHD, ghd)):
            fw.dma("sp", tl[:], src.h.partition_broadcast(128), reads=[src], writes=[tl])
        MG = S_("MG", [128, 4, NB]); LF = S_("LF", [128, 2, NB])
        for c in range(4):
            TS("dve", MG[:, c, :], SMA[:, :, c], GB[:, c:c + 1], ALU.add, [SMA, GB], [MG])
        ACT(MG[:], MG[:], AF.Tanh, [MG], [MG], scale=1.0 / 15.0)
        TS("dve", MG[:], MG[:], 15.0, ALU.mult, [MG], [MG])
        for d in range(2):
            ACT(LF[:, d, :], MG[:, 2 * d + 1, :], AF.Exp, [MG], [LF], scale=-1.0)
        TS("dve", LF[:], LF[:], 1.0, ALU.add, [LF], [LF])
        ACT(LF[:], LF[:], AF.Ln, [LF], [LF])
        TS("dve", LF[:], LF[:], -1.0, ALU.mult, [LF], [LF])
        GZ = S_("GZ", [128, 4, NB]); T1 = S_("T1", [128, 4, NB]); GG = S_("GG", [128, 4, NB])
        BB = S_("BB", [128, 4, NB]); LNB = S_("LNB", [128, 4, NB]); BETA = S_("BETA", [128, 4, NB])
        NEA = S_("NEA", [128, 4])
        ACT(NEA[:], AL[:], AF.Exp, [AL], [NEA])
        TS("dve", NEA[:], NEA[:], -1.0, ALU.mult, [NEA], [NEA])
        for c in range(4):
            TS("dve", GZ[:, c, :], SMA[:, :, 4 + c], DT[:, c:c + 1], ALU.add, [SMA, DT], [GZ])
            CP("dve", BB[:, c, :], SMA[:, :, 8 + c], [SMA], [BB])
        def softplus_neg_abs(dst, src):
            TS("dve", dst, src, 0.0, ALU.abs_max, [GZ, BB], [T1, LNB])
        STT("dve", T1[:], GZ[:], -1.0, GZ[:], ALU.mult, ALU.max, [GZ], [T1])
        ACT(T1[:], T1[:], AF.Exp, [T1], [T1], scale=-1.0)
        TS("dve", T1[:], T1[:], 1.0, ALU.add, [T1], [T1])
        ACT(T1[:], T1[:], AF.Ln, [T1], [T1])
        STT("dve", T1[:], GZ[:], 0.0, T1[:], ALU.max, ALU.add, [GZ, T1], [T1])
        for c in range(4):
            TS("dve", GG[:, c, :], T1[:, c, :], NEA[:, c:c + 1], ALU.mult, [T1, NEA], [GG])
        STT("dve", LNB[:], BB[:], -1.0, BB[:], ALU.mult, ALU.max, [BB], [LNB])
        ACT(LNB[:], LNB[:], AF.Exp, [LNB], [LNB], scale=-1.0)
        TS("dve", LNB[:], LNB[:], 1.0, ALU.add, [LNB], [LNB])
        ACT(LNB[:], LNB[:], AF.Ln, [LNB], [LNB])
        STT("dve", LNB[:], BB[:], 0.0, LNB[:], ALU.min, ALU.subtract, [BB, LNB], [LNB])
        ACT(BETA[:], LNB[:], AF.Exp, [LNB], [BETA])

        def mk(names, shape=(128, 128)):
            return {n: S_(n, list(shape)) for n in names}
        fmL = [S_("fmL%d" % i, [128, 2, 128]) for i in range(2)]
        tmL = [S_("tmL%d" % i, [128, 385]) for i in range(2)]
        gfL = [S_("gfL%d" % i, [128, 4, 128]) for i in range(2)]
        gtL = [S_("gtL%d" % i, [128, 4, 128]) for i in range(2)]
        h0L = [S_("h0L%d" % i, [128, 512]) for i in range(2)]
        ogL = [S_("ogL%d" % i, [128, 512]) for i in range(2)]
        hs = [S_("hs%d" % i, [128, 512]) for i in range(2)]
        yt = [S_("yt%d" % i, [128, 512], BF16) for i in range(2)]
        jk = S_("jk", [128, 256])
        m_ = mk(["lfb", "WTm", "PT", "E0", "qe", "kw"]); m_.update(mk(["bt", "bs", "ws", "dec", "dn"], (128, 2)))
        CTs = S_("CTs", [128, 257])
        g_ = []
        for h in range(2):
            gd = mk(["gb%d" % h, "ngb%d" % h, "lb%d" % h, "DA%d" % h, "EAT%d" % h, "EA%d" % h, "AT%d" % h, "A%d" % h, "AttT%d" % h, "X%d" % h, "XT%d" % h,
                     "Bs%d" % h, "BTs%d" % h, "Y%d" % h, "Yp%d" % h, "Ru%d" % h, "Rw%d" % h, "kd%d" % h, "nwT%d" % h, "vn%d" % h, "E0%d" % h, "qg%d" % h, "S%d" % h])
            gd = {k[:-1]: v for k, v in gd.items()}
            gd.update({k: S_(k + str(h), [128, 2]) for k in ["bt", "ngc", "gl", "bw", "kds", "dec"]})
            g_.append(gd)
        ssn = S_("ssn", [128, 4]); rsn = S_("rsn", [128, 4])
        cnt = [0]

        def mlstm_block(d, blk, is_lat, fm, tm, hsb, h0t):
            li = MG[:, 2 * d, blk:blk + 1]; lf = LF[:, d, blk:blk + 1]
            qT, kT = fm[:, 0, :], fm[:, 1, :]; kTM, v1 = tm[:, 0:128], tm[:, 128:385]
            M = m_
            TS("dve", M["lfb"][:], ones[:], lf, ALU.mult, [ones, LF], [M["lfb"]])
            b0 = nb_()
            MM(b0[:, 0:128], M["lfb"][:], TRI[d], True, True, [M["lfb"], CONST], [b0])
            MM(b0[:, 128:129], TRI[d], lf, True, True, [CONST, LF], [b0])
            MM(b0[:, 129:130], ones[:], lf, True, True, [ones, LF], [b0])
            CP("dve", M["bt"][:], b0[:, 128:130], [b0], [M["bt"]])
            TT("dve", M["bs"][:, 0:1], li, M["bt"][:, 0:1], ALU.subtract, [MG, M["bt"]], [M["bs"]])
            if is_lat:
                b1 = nb_()
                MM(b1[:, 0:128], M["lfb"][:], TRI[d], True, False, [M["lfb"], CONST], [b1])
                MM(b1[:, 0:128], ident, NI[d], False, True, [CONST], [b1])
                ACT(M["WTm"][:], b1[:, 0:128], AF.Exp, [b1, M["bs"]], [M["WTm"]], bias=M["bs"][:, 0:1])
                b2 = nb_()
                MM(b2[:, 0:128], kT, qT, True, True, [fm], [b2])
                TT("dve", M["PT"][:], b2[:, 0:128], M["WTm"][:], ALU.mult, [b2, M["WTm"]], [M["PT"]])
                ACT(M["E0"][:], b0[:, 0:128], AF.Exp, [b0], [M["E0"]])
                TT("dve", M["qe"][:], qT, M["E0"][:], ALU.mult, [fm, M["E0"]], [M["qe"]])
                b3 = nb_()
                MM(b3[:, 0:257], M["PT"][:], v1, True, False, [M["PT"], tm], [b3])
                MM(b3[:, 0:257], M["qe"][:], CTs[:], False, True, [M["qe"], CTs], [b3])
                TS("dve", M["dn"][:, 1:2], b3[:, 256:257], -1.0, ALU.mult, [b3], [M["dn"]], s2=1.0, op1=ALU.max)
                TS("dve", M["dn"][:, 0:1], b3[:, 256:257], 1.0, ALU.max, [b3], [M["dn"]])
                TT("dve", M["dn"][:, 0:1], M["dn"][:, 0:1], M["dn"][:, 1:2], ALU.max, [M["dn"]], [M["dn"]])
                fw.op("dve", lambda e: e.reciprocal(out=M["dn"][:, 0:1], in_=M["dn"][:, 0:1]), reads=[M["dn"]], writes=[M["dn"]])
                if d == 0:
                    TS("dve", hsb[:, 0:256], b3[:, 0:256], M["dn"][:, 0:1], ALU.mult, [b3, M["dn"]], [hsb])
                else:
                    STT("dve", hsb[:, 0:256], b3[:, 0:256], M["dn"][:, 0:1], h0t[:, 0:256], ALU.mult, ALU.add, [b3, M["dn"], h0t], [hsb])
            ACT(M["ws"][:, 0:1], M["bs"][:, 0:1], AF.Exp, [M["bs"], M["bt"]], [M["ws"]], bias=M["bt"][:, 1:2])
            TS("dve", M["kw"][:], kTM, M["ws"][:, 0:1], ALU.mult, [tm, M["ws"]], [M["kw"]])
            b4 = nb_()
            MM(b4[:, 0:257], M["kw"][:], v1, True, True, [M["kw"], tm], [b4])
            ACT(M["dec"][:, 0:1], M["bt"][:, 1:2], AF.Exp, [M["bt"]], [M["dec"]])
            STT("dve", CTs[:], CTs[:], M["dec"][:, 0:1], b4[:, 0:257], ALU.mult, ALU.add, [CTs, M["dec"], b4], [CTs])

        def gdn_block(d, h, blk, is_lat, gf, gt, hsb, h0t):
            G = g_[h]; c = 2 * d + h
            g = GG[:, c, blk:blk + 1]; lnb = LNB[:, c, blk:blk + 1]; beta = BETA[:, c, blk:blk + 1]
            qT, kT, kTM, vTM = gf[:, h, :], gf[:, 2 + h, :], gt[:, h, :], gt[:, 2 + h, :]
            TS("dve", G["gb"][:], ones[:], g, ALU.mult, [ones, GG], [G["gb"]])
            TS("dve", G["ngb"][:], G["gb"][:], -1.0, ALU.mult, [G["gb"]], [G["ngb"]])
            TS("dve", G["lb"][:], ones[:], lnb, ALU.mult, [ones, LNB], [G["lb"]])
            b0 = nb_()
            MM(b0[:, 0:128], G["gb"][:], TRI[d], True, True, [G["gb"], CONST], [b0])
            MM(b0[:, 128:129], TRI[d], g, True, True, [CONST, GG], [b0])
            MM(b0[:, 129:130], ones[:], g, True, True, [ones, GG], [b0])
            CP("dve", G["bt"][:], b0[:, 128:130], [b0], [G["bt"]])
            if is_lat:
                ACT(G["E0"][:], b0[:, 0:128], AF.Exp, [b0], [G["E0"]])
            TS("dve", G["ngc"][:, 0:1], G["bt"][:, 0:1], -1.0, ALU.mult, [G["bt"]], [G["ngc"]])
            TT("dve", G["gl"][:, 0:1], G["bt"][:, 0:1], lnb, ALU.add, [G["bt"], LNB], [G["gl"]])
            if is_lat:
                b1 = nb_()
                MM(b1[:, 0:128], G["gb"][:], TRI[d], True, False, [G["gb"], CONST], [b1])
                MM(b1[:, 0:128], ident, NI[d], False, True, [CONST], [b1])
                ACT(G["DA"][:], b1[:, 0:128], AF.Exp, [b1, G["ngc"]], [G["DA"]], bias=G["ngc"][:, 0:1])
            b2 = nb_()
            MM(b2[:, 0:128], G["gb"][:], TRI[d], True, False, [G["gb"], CONST], [b2])
            MM(b2[:, 0:128], G["lb"][:], ident, False, False, [G["lb"], CONST], [b2])
            MM(b2[:, 0:128], ident, NS[d], False, True, [CONST], [b2])
            ACT(G["EAT"][:], b2[:, 0:128], AF.Exp, [b2, G["ngc"]], [G["EAT"]], bias=G["ngc"][:, 0:1])
            b3 = nb_()
            MM(b3[:, 0:128], G["ngb"][:], TRI[d], True, False, [G["ngb"], CONST], [b3])
            MM(b3[:, 0:128], ident, NST[d], False, True, [CONST], [b3])
            ACT(G["EA"][:], b3[:, 0:128], AF.Exp, [b3, G["gl"]], [G["EA"]], bias=G["gl"][:, 0:1])
            b4 = nb_()
            MM(b4[:, 0:128], kT, kT, True, True, [gf], [b4])
            TT("dve", G["AT"][:], b4[:, 0:128], G["EAT"][:], ALU.mult, [b4, G["EAT"]], [G["AT"]])
            TT("dve", G["A"][:], b4[:, 0:128], G["EA"][:], ALU.mult, [b4, G["EA"]], [G["A"]])
            if is_lat:
                b5 = nb_()
                MM(b5[:, 0:128], kT, qT, True, True, [gf], [b5])
                TT("dve", G["AttT"][:], b5[:, 0:128], G["DA"][:], ALU.mult, [b5, G["DA"]], [G["AttT"]])
            TT("dve", G["Bs"][:], G["A"][:], MK(d, 0), ALU.mult, [G["A"], CONST], [G["Bs"]])
            TT("dve", G["X"][:], ident, G["Bs"][:], ALU.subtract, [CONST, G["Bs"]], [G["X"]])
            TT("dve", G["BTs"][:], G["AT"][:], MKT(d, 0), ALU.mult, [G["AT"], CONST], [G["BTs"]])
            TT("dve", G["XT"][:], ident, G["BTs"][:], ALU.subtract, [CONST, G["BTs"]], [G["XT"]])
            for li in range(1, 7):
                last = li == 6
                TT("dve", G["Bs"][:], G["A"][:], MK(d, li), ALU.mult, [G["A"], CONST], [G["Bs"]])
                pyp = nb_()
                MM(pyp[:, 0:128], G["Bs"][:], G["XT"][:], True, True, [G["Bs"], G["XT"]], [pyp])
                CP("act", G["Yp"][:], pyp[:, 0:128], [pyp], [G["Yp"]])
                pzp = nb_()
                MM(pzp[:, 0:128], G["X"][:], G["Yp"][:], True, True, [G["X"], G["Yp"]], [pzp])
                if not last:
                    TT("dve", G["BTs"][:], G["AT"][:], MKT(d, li), ALU.mult, [G["AT"], CONST], [G["BTs"]])
                    py = nb_()
                    MM(py[:, 0:128], G["BTs"][:], G["X"][:], True, True, [G["BTs"], G["X"]], [py])
                    CP("act", G["Y"][:], py[:, 0:128], [py], [G["Y"]])
                    pz = nb_()
                    MM(pz[:, 0:128], G["XT"][:], G["Y"][:], True, True, [G["XT"], G["Y"]], [pz])
                    TT("dve", G["X"][:], G["X"][:], pz[:, 0:128], ALU.subtract, [G["X"], pz], [G["X"]])
                TT("dve", G["XT"][:], G["XT"][:], pzp[:, 0:128], ALU.subtract, [G["XT"], pzp], [G["XT"]])
            ACT(G["bw"][:, 0:1], G["gl"][:, 0:1], AF.Exp, [G["gl"]], [G["bw"]])
            TS("dve", G["Ru"][:], vTM, beta, ALU.mult, [gt, BETA], [G["Ru"]])
            TS("dve", G["Rw"][:], kTM, G["bw"][:, 0:1], ALU.mult, [gt, G["bw"]], [G["Rw"]])
            ACT(G["kds"][:, 0:1], G["bt"][:, 0:1], AF.Exp, [G["bt"]], [G["kds"]], scale=-1.0, bias=G["bt"][:, 1:2])
            TS("dve", G["kd"][:], kTM, G["kds"][:, 0:1], ALU.mult, [gt, G["kds"]], [G["kd"]])
            pw = nb_()
            MM(pw[:, 0:128], G["Rw"][:], G["XT"][:], True, True, [G["Rw"], G["XT"]], [pw])
            fw.op("act", lambda e: e.mul(out=G["nwT"][:], in_=pw[:, 0:128], mul=-1.0), reads=[pw], writes=[G["nwT"]])
            pv = nb_()
            MM(pv[:, 0:128], G["XT"][:], G["Ru"][:], True, False, [G["XT"], G["Ru"]], [pv])
            MM(pv[:, 0:128], G["nwT"][:], G["S"][:], False, True, [G["nwT"], G["S"]], [pv])
            CP("act", G["vn"][:], pv[:, 0:128], [pv], [G["vn"]])
            if is_lat:
                TT("dve", G["qg"][:], qT, G["E0"][:], ALU.mult, [gf, G["E0"]], [G["qg"]])
                po = nb_()
                MM(po[:, 0:128], G["qg"][:], G["S"][:], True, False, [G["qg"], G["S"]], [po])
                MM(po[:, 0:128], G["AttT"][:], G["vn"][:], False, True, [G["AttT"], G["vn"]], [po])
                cs = slice(256 + h * 128, 256 + (h + 1) * 128)
                if d == 0:
                    CP("dve", hsb[:, cs], po[:, 0:128], [po], [hsb])
                else:
                    TT("dve", hsb[:, cs], po[:, 0:128], h0t[:, cs], ALU.add, [po, h0t], [hsb])
            pu = nb_()
            MM(pu[:, 0:128], G["kd"][:], G["vn"][:], True, True, [G["kd"], G["vn"]], [pu])
            ACT(G["dec"][:, 0:1], G["bt"][:, 1:2], AF.Exp, [G["bt"]], [G["dec"]])
            STT("dve", G["S"][:], G["S"][:], G["dec"][:, 0:1], pu[:, 0:128], ALU.mult, ALU.add, [G["S"], G["dec"], pu], [G["S"]])

        for d in range(2):
            fw.op("dve", lambda e: e.memset(CTs[:], 0.0), writes=[CTs])
            for h in range(2):
                fw.op("dve", lambda e, h=h: e.memset(g_[h]["S"][:], 0.0), writes=[g_[h]["S"]])
            order = [NBL, NBL + 1] + list(range(NBL)) if d == 0 else [NBL + 1, NBL] + list(range(NBL - 1, -1, -1))
            for blk in order:
                i = cnt[0]; cnt[0] += 1
                is_lat = blk < NBL
                fm, tm, gf, gt = fmL[i % 2], tmL[i % 2], gfL[i % 2], gtL[i % 2]
                hsb, h0t, og, yb = hs[i % 2], h0L[i % 2], ogL[i % 2], yt[i % 2]
                fw.dma("sp", fm[:], SM_FM.h[blk], reads=[SM_FM], writes=[fm])
                fw.dma("sp", tm[:], SM_TM.h[blk], reads=[SM_TM], writes=[tm])
                fw.dma("sp", gf[:], SG_FM.h[blk], reads=[SG_FM], writes=[gf])
                fw.dma("sp", gt[:], SG_TM.h[blk], reads=[SG_TM], writes=[gt])
                if is_lat and d == 1:
                    fw.dma("sp", h0t[:], H0.h[blk], reads=[H0], writes=[h0t])
                    fw.dma("sp", og[:], OG.h[blk], reads=[OG], writes=[og])
                mlstm_block(d, blk, is_lat, fm, tm, hsb, h0t)
                gdn_block(d, 0, blk, is_lat, gf, gt, hsb, h0t)
                gdn_block(d, 1, blk, is_lat, gf, gt, hsb, h0t)
                if not is_lat:
                    continue
                if d == 0:
                    fw.dma("sp", H0.h[blk], hsb[:], reads=[hsb], writes=[H0])
                    continue
                segs = [(0, 256, GHM[:, 0:256]), (256, 384, GHD[:, 0:128]), (384, 512, GHD[:, 128:256])]
                for si, (lo, hi, gsc) in enumerate(segs):
                    ACT(jk[:, 0:hi - lo], hsb[:, lo:hi], AF.Square, [hsb], [jk, ssn], accum=ssn[:, si:si + 1])
                    ACT(rsn[:, si:si + 1], ssn[:, si:si + 1], AF.Sqrt, [ssn, epsT], [rsn], bias=epsT[:], scale=1.0 / (hi - lo))
                fw.op("dve", lambda e: e.reciprocal(out=rsn[:, 0:3], in_=rsn[:, 0:3]), reads=[rsn], writes=[rsn])
                ACT(og[:, 0:256], og[:, 0:256], AF.Sigmoid, [og], [og])
                ACT(og[:, 256:512], og[:, 256:512], AF.Silu, [og], [og])
                for si, (lo, hi, gsc) in enumerate(segs):
                    STT("dve", hsb[:, lo:hi], hsb[:, lo:hi], rsn[:, si:si + 1], gsc, ALU.mult, ALU.mult, [hsb, rsn, GHM, GHD], [hsb])
                TT("dve", yb[:], hsb[:], og[:], ALU.mult, [hsb, og], [yb])
                fw.dma("sp", y.h[blk * 128:(blk + 1) * 128, :], yb[:], reads=[yb], writes=[y], is_output=True)

M_QK, M_V, G_W = 512, 1024, 1024
OFF = {}
o = 0
for nm, w in [("mq", 512), ("mk", 512), ("mv", 1024), ("mo", 1024), ("mg", 16), ("gqkv", 3072), ("go", 1024), ("ga", 16), ("gb", 16)]:
    OFF[nm] = o; o += w

def l1_inputs(inp, core, NBL=64):
    b, hg = core // 4, core % 4
    hm = hg; h0, h1 = 2 * hg, 2 * hg + 1
    w_in = inp["w_in"][0]
    def cols(base, start, n): return list(range(OFF[base] + start, OFF[base] + start + n))
    gq = lambda h: cols("gqkv", h * 128, 128)
    gk = lambda h: cols("gqkv", G_W + h * 128, 128)
    gv = lambda h: cols("gqkv", 2 * G_W + h * 128, 128)
    fcols = cols("mq", hm * 128, 128) + cols("mk", hm * 128, 128) + gq(h0) + gq(h1) + gk(h0) + gk(h1) + gv(h0) + gv(h1)
    mg = [OFF["mg"] + d * 8 + t * 4 + hm for d in range(2) for t in range(2)]
    ga = [OFF["ga"] + d * 8 + h for d in range(2) for h in (h0, h1)]
    gb = [OFF["gb"] + d * 8 + h for d in range(2) for h in (h0, h1)]
    tcols = cols("mk", hm * 128, 128) + cols("mv", hm * 256, 256) + cols("go", h0 * 128, 128) + cols("mo", hm * 256, 256) + cols("go", h1 * 128, 128) + mg + ga + gb
    wc = inp["w_conv"][0]
    ccols = [c - OFF["gqkv"] for c in gq(h0) + gq(h1) + gk(h0) + gk(h1) + gv(h0) + gv(h1)]
    wconv = np.ascontiguousarray(wc[:, ccols].reshape(5, 6, 128).transpose(2, 1, 0))
    bg = inp["b_gate_m"][0]
    return {
        "x": np.ascontiguousarray(inp["x"][b, :NBL * 128]), "ctx": np.ascontiguousarray(inp["ctx"][b]),
        "c_l": np.ascontiguousarray(inp["c"][b].reshape(16, 128).T), "cc_l": np.ascontiguousarray(inp["c_ctx"].reshape(16, 128).T),
        "w_ada1": np.ascontiguousarray(inp["w_ada"][0][:, :4096]), "b_ada1": np.ascontiguousarray(inp["b_ada"][0][None, :4096]),
        "gn1": inp["g_norm1"][0],
        "wF": np.ascontiguousarray(w_in[:, fcols]), "wT": np.ascontiguousarray(w_in[:, tcols]),
        "wconv": wconv,
        "gbias": np.ascontiguousarray(np.array([bg[d, t, hm] for d in range(2) for t in range(2)], np.float32)),
        "alog": np.ascontiguousarray(np.array([inp["a_log"][0][d, h] for d in range(2) for h in (h0, h1)], np.float32)),
        "dtb": np.ascontiguousarray(np.array([inp["dt_bias"][0][d, h] for d in range(2) for h in (h0, h1)], np.float32)),
        "ghm": np.ascontiguousarray(inp["g_head_m"][0][hm * 256:(hm + 1) * 256]),
        "ghd": np.ascontiguousarray(inp["g_head_d"][0][h0 * 128:(h1 + 1) * 128]),
        "constd": l1_consts(),
    }

def ycols(core):
    hg = core % 4
    return list(range(hg * 256, (hg + 1) * 256)) + list(range(1024 + 2 * hg * 128, 1024 + (2 * hg + 2) * 128))


def l2_inputs(inp, ycat, core):
    b, j = core // 4, core % 4
    ts = slice(j * 2048, (j + 1) * 2048)
    return {
        "x": np.ascontiguousarray(inp["x"][b, ts]),
        "yT": np.ascontiguousarray(ycat[b, ts].T),
        "c_l": np.ascontiguousarray(inp["c"][b].reshape(16, 128).T),
        "w_ada2": np.ascontiguousarray(inp["w_ada"][0][:, 2 * 2048:]),
        "b_ada2": np.ascontiguousarray(inp["b_ada"][0][None, 2 * 2048:]),
        "gn2": inp["g_norm2"][0], "gfin": inp["g_final"],
        "w_out": inp["w_out"][0],
        "wr": np.ascontiguousarray(np.concatenate([inp["w_grp"][0], inp["w_rtr"][0]], axis=1)),
        "br": np.ascontiguousarray(np.concatenate([inp["b_grp"][0], inp["b_rtr"][0]])[None, :]),
        "w1": inp["w1"][0], "w3": inp["w3"][0], "w2": inp["w2"][0],
        "identd": np.eye(128, dtype=np.float32),
    }


def _launch(nc, maps):
    res = []
    for g in range(0, len(maps), 4):
        res += run_bass_kernel_spmd(nc, maps[g:g + 4], core_ids=list(range(4))).results
    return res


def kernel(**inputs):
    inp = {k: np.asarray(v) for k, v in inputs.items()}
    nc1 = build_l1(NBL=64)
    r1 = _launch(nc1, [l1_inputs(inp, c, 64) for c in range(8)])
    ycat = np.zeros((2, 8192, 2048), dtype=ml_dtypes.bfloat16)
    for c in range(8):
        ycat[c // 4][:, ycols(c)] = r1[c]["y"]
    nc2 = build_l2()
    r2 = _launch(nc2, [l2_inputs(inp, ycat, c) for c in range(8)])
    out = np.zeros((2, 8192, 2048), dtype=np.float32)
    for c in range(8):
        out[c // 4, (c % 4) * 2048:(c % 4 + 1) * 2048] = r2[c]["out"]
    return out
```

```python
import numpy as np
import contextlib
import ml_dtypes
import concourse.bass as bass
import concourse.mybir as mybir
from concourse.bass_utils import run_bass_kernel_spmd

F32 = mybir.dt.float32
BF16 = mybir.dt.bfloat16
I32 = mybir.dt.int32
ALU = mybir.AluOpType
AF = mybir.ActivationFunctionType
AX = mybir.AxisListType

SEM_LIMIT = 20000
N_DMA_SEMS = 24


class Tile:
    __slots__ = ("name", "h", "last_w", "readers", "excl")

    def __init__(self, name, h):
        self.name = name
        self.h = h
        self.last_w = None
        self.readers = []
        self.excl = False

    def __getitem__(self, k):
        return self.h[k]


class FW:
    def __init__(self, nc, stack):
        self.nc = nc
        self.stack = stack
        self.eng = {"pe": nc.tensor, "act": nc.scalar, "dve": nc.vector, "pool": nc.gpsimd, "sp": nc.sync}
        self.sem = {}
        self.cnt = {}
        self.pe_sems = []
        self.nsem = 0
        for e in self.eng:
            self._new_sem(e)
        self.seen = {e: {} for e in self.eng}
        self.dma_sems = [self._mk_sem("dma%d" % i) for i in range(2 * N_DMA_SEMS)]
        self.dma_cnt = [0] * (2 * N_DMA_SEMS)
        self.dma_i = [0, 0]
        self.n_tiles = 0
        self.out_tokens = []

    def _mk_sem(self, name):
        self.nsem += 1
        return self.stack.enter_context(self.nc.semaphore("%s_%d" % (name, self.nsem)))

    def _new_sem(self, e):
        self.sem[e] = self._mk_sem("s_" + e)
        self.cnt[e] = 0
        if e == "pe":
            self.pe_sems.append(self.sem[e])

    def sb(self, name, shape, dt=F32):
        self.n_tiles += 1
        h = self.nc.alloc_sbuf_tensor("%s_%d" % (name, self.n_tiles), list(shape), dt)
        return Tile(name, h)

    def ps(self, name, shape, dt=F32):
        self.n_tiles += 1
        h = self.stack.enter_context(self.nc.psum_tensor("%s_%d" % (name, self.n_tiles), list(shape), dt))
        t = Tile(name, h)
        t.excl = True
        return t

    def dram(self, name, shape, dt=F32, kind="Internal"):
        h = self.nc.dram_tensor(name, list(shape), dt, kind=kind)
        return Tile(name, h.ap())

    def _wait(self, e, tok):
        if tok is None:
            return
        sem, val = tok
        if e == "pe" and sem in self.pe_sems:
            return
        k = id(sem)
        if self.seen[e].get(k, 0) >= val:
            return
        self.seen[e][k] = val
        self.eng[e].wait_ge(sem, val)

    def _deps(self, e, reads, writes):
        for t in reads:
            self._wait(e, t.last_w)
            if t.excl:
                for r in t.readers:
                    self._wait(e, r)
        for t in writes:
            self._wait(e, t.last_w)
            for r in t.readers:
                self._wait(e, r)

    def _commit(self, tok, reads, writes):
        for t in reads:
            t.readers.append(tok)
            if len(t.readers) > 64:
                t.readers = t.readers[-64:]
        for t in writes:
            t.last_w = tok
            t.readers = []

    def op(self, e, fn, reads=(), writes=()):
        if self.cnt[e] >= SEM_LIMIT:
            self._new_sem(e)
        self._deps(e, reads, writes)
        ins = fn(self.eng[e])
        self.cnt[e] += 1
        ins.then_inc(self.sem[e], 1)
        tok = (self.sem[e], self.cnt[e])
        self._commit(tok, reads, writes)
        return tok

    def dma(self, q, out, in_, reads=(), writes=(), is_output=False, **kw):
        sw = 1 if q == "pool" else 0
        r = sw * N_DMA_SEMS + self.dma_i[sw] % N_DMA_SEMS
        self.dma_i[sw] += 1
        sem = self.dma_sems[r]
        if self.dma_cnt[r] > 0:
            self._wait(q, (sem, self.dma_cnt[r]))
        self._deps(q, reads, writes)
        ins = self.eng[q].dma_start(out=out, in_=in_, **kw)
        self.dma_cnt[r] += 16
        ins.then_inc(sem, 16)
        tok = (sem, self.dma_cnt[r])
        self._commit(tok, reads, writes)
        if is_output:
            self.out_tokens.append(tok)
        return tok

    def barrier(self):
        toks = [(self.sem[e], self.cnt[e]) for e in self.eng if self.cnt[e] > 0]
        toks += [(self.dma_sems[r], self.dma_cnt[r]) for r in range(2 * N_DMA_SEMS) if self.dma_cnt[r] > 0]
        for e in self.eng:
            for t in toks:
                if t[0] is self.sem[e]:
                    continue
                self._wait(e, t)

    def finish(self):
        for r in range(2 * N_DMA_SEMS):
            if self.dma_cnt[r] > 0:
                self._wait("sp", (self.dma_sems[r], self.dma_cnt[r]))
        for e in self.eng:
            if e != "sp" and self.cnt[e] > 0:
                self._wait("sp", (self.sem[e], self.cnt[e]))


D = 2048
NT2 = 16
NE = 32
DE = 768
EPS = 1e-6


def sbs(fw, stack, name, shape, dt=F32):
    fw.n_tiles += 1
    h = stack.enter_context(fw.nc.sbuf_tensor("%s_%d" % (name, fw.n_tiles), list(shape), dt))
    return Tile(name, h)


def build_l2(n_experts=NE, n_pass=2, stop=None, nt_d=NT2, cut=9):
    nc = bass.Bass("TRN2", target_bir_lowering=False)
    TOK = 2048
    with contextlib.ExitStack() as st:
        fw = FW(nc, st)
        def din(name, shape, dt=F32):
            return Tile(name, nc.dram_tensor(name, list(shape), dt, kind="ExternalInput").ap())
        x = din("x", [TOK, D])
        yT = din("yT", [D, TOK], BF16)
        c_l = din("c_l", [128, 16])
        w_ada2 = din("w_ada2", [D, 4 * D])
        b_ada2 = din("b_ada2", [1, 4 * D])
        gn2 = din("gn2", [D])
        gfin = din("gfin", [D])
        w_out = din("w_out", [D, D])
        wr = din("wr", [D, 36])
        br = din("br", [1, 36])
        if n_experts > 0:
            w1 = din("w1", [NE, D, DE])
            w3 = din("w3", [NE, D, DE])
            w2 = din("w2", [NE, DE, D])
        out = Tile("out", nc.dram_tensor("out", [TOK, D], F32, kind="ExternalOutput").ap())
        X1 = fw.dram("x1_scr", [TOK, D], F32)
        H2T = fw.dram("h2t_scr", [128, 16, TOK], BF16)

        ident = sbs(fw, st, "ident", [128, 128], F32)
        ones = sbs(fw, st, "ones", [128, 128], F32)
        epsT = sbs(fw, st, "eps", [128, 1], F32)
        GT2 = sbs(fw, st, "GT2", [128, D])
        GFIN = sbs(fw, st, "GFIN", [128, D])
        WT = sbs(fw, st, "WT", [128, NT2, NE])
        P = [fw.ps("P%d" % i, [128, 512], F32) for i in range(8)]

        fw.op("dve", lambda e: e.memset(ones[:], 1.0), writes=[ones])
        fw.op("dve", lambda e: e.memset(epsT[:], EPS), writes=[epsT])
        identd = din("identd", [128, 128])
        fw.dma("sp", ident[:], identd.h, reads=[identd], writes=[ident])
        fw.dma("sp", GFIN[:], gfin.h.partition_broadcast(128), reads=[gfin], writes=[GFIN])

        with contextlib.ExitStack() as sAD:
            GT1 = sbs(fw, sAD, "GT1", [128, D])
            G2 = sbs(fw, sAD, "G2", [128, D])
            SH2 = sbs(fw, sAD, "SH2", [128, D])
            with contextlib.ExitStack() as sA:
                csl = sbs(fw, sA, "csl", [128, 16])
                CB = sbs(fw, sA, "CB", [128, 16, 128])
                brow = sbs(fw, sA, "brow", [1, 4 * D])
                GN2 = sbs(fw, sA, "GN2", [128, D])
                Wst = [sbs(fw, sA, "Wst%d" % i, [128, 4096]) for i in range(2)]
                fw.dma("sp", csl[:], c_l.h, reads=[c_l], writes=[csl])
                fw.dma("sp", brow[:], b_ada2.h, reads=[b_ada2], writes=[brow])
                fw.dma("sp", GN2[:], gn2.h.partition_broadcast(128), reads=[gn2], writes=[GN2])
                fw.op("act", lambda e: e.activation(out=csl[:], in_=csl[:], func=AF.Silu), reads=[csl], writes=[csl])
                for k in range(16):
                    fw.op("dve", lambda e, k=k: e.tensor_scalar(out=CB[:, k, :], in0=ones[:], scalar1=csl[:, k:k + 1], scalar2=None, op0=ALU.mult),
                          reads=[ones, csl], writes=[CB])
                li = 0
                for half in range(2):
                    for k in range(16):
                        wt = Wst[li % 2]; li += 1
                        fw.dma("sp", wt[:], w_ada2.h[k * 128:(k + 1) * 128, half * 4096:(half + 1) * 4096], reads=[w_ada2], writes=[wt])
                        for j in range(8):
                            fw.op("pe", lambda e, j=j, k=k, wt=wt: e.matmul(P[j][:], lhsT=CB[:, k, :], rhs=wt[:, j * 512:(j + 1) * 512], start=(k == 0), stop=False),
                                  reads=[CB, wt], writes=[P[j]])
                    for j in range(8):
                        fw.op("pe", lambda e, j=j, half=half: e.matmul(P[j][:], lhsT=ones[0:1, :], rhs=brow[0:1, half * 4096 + j * 512: half * 4096 + (j + 1) * 512], start=False, stop=True),
                              reads=[ones, brow], writes=[P[j]])
                    for j in range(8):
                        cs = slice((j % 4) * 512, (j % 4 + 1) * 512)
                        if half == 0:
                            dst = GT1 if j < 4 else SH2
                            fw.op("act", lambda e, j=j, dst=dst, cs=cs: e.copy(out=dst[:, cs], in_=P[j][:]), reads=[P[j]], writes=[dst])
                        elif j < 4:
                            fw.op("dve", lambda e, j=j, cs=cs: e.scalar_tensor_tensor(out=G2[:, cs], in0=P[j][:], scalar=1.0, in1=GN2[:, cs], op0=ALU.add, op1=ALU.mult),
                                  reads=[P[j], GN2], writes=[G2])
                        else:
                            fw.op("act", lambda e, j=j, cs=cs: e.copy(out=GT2[:, cs], in_=P[j][:]), reads=[P[j]], writes=[GT2])
                fw.barrier()
                if stop == "A":
                    fw.dma("sp", out.h[0:128, :], GT1[:], reads=[GT1], writes=[out], is_output=True)
                    fw.dma("sp", out.h[128:256, :], G2[:], reads=[G2], writes=[out], is_output=True)
                    fw.dma("sp", out.h[256:384, :], SH2[:], reads=[SH2], writes=[out], is_output=True)
                    fw.dma("sp", out.h[384:512, :], GT2[:], reads=[GT2], writes=[out], is_output=True)
                    fw.finish()
                    return nc
            with contextlib.ExitStack() as sD:
                WO = sbs(fw, sD, "WO", [128, 16, D], BF16)
                WR = sbs(fw, sD, "WR", [128, 16, 36])
                brr = sbs(fw, sD, "brr", [1, 36])
                xt = [sbs(fw, sD, "xt%d" % i, [128, D]) for i in range(2)]
                yt = [sbs(fw, sD, "yt%d" % i, [128, 16, 128], BF16) for i in range(2)]
                tmp = [sbs(fw, sD, "tmp%d" % i, [128, D]) for i in range(2)]
                hTf = sbs(fw, sD, "hTf", [128, 16, 128])
                hTb = [sbs(fw, sD, "hTb%d" % i, [128, 16, 128], BF16) for i in range(2)]
                sm = {n: sbs(fw, sD, n, [128, w]) for n, w in
                      [("ss", 1), ("rstd", 1), ("lg", 36), ("gmax", 1), ("ngmax", 1), ("eg", 4), ("sumg", 1), ("pgrp", 1), ("oh", 4), ("pen", 4),
                       ("lm", 32), ("top8", 8), ("sel", 32), ("nm1", 1), ("ew", 32), ("den", 1), ("fac", 1)]}
                fw.dma("pool", WO[:], w_out.h.rearrange("(k p) n -> p k n", p=128), reads=[w_out], writes=[WO])
                fw.dma("sp", WR[:], wr.h.rearrange("(k p) n -> p k n", p=128), reads=[wr], writes=[WR])
                fw.dma("sp", brr[:], br.h, reads=[br], writes=[brr])
                yTv = yT.h.rearrange("(k p) t -> p k t", p=128)
                for t in range(nt_d):
                    xb, yb, t1, t2 = xt[t % 2], yt[t % 2], tmp[0], tmp[1]
                    ts = slice(t * 128, (t + 1) * 128)
                    fw.dma("sp", xb[:], x.h[ts, :], reads=[x], writes=[xb])
                    fw.dma("sp", yb[:], yTv[:, :, ts], reads=[yT], writes=[yb])
                    for nb in range(4):
                        for k in range(16):
                            fw.op("pe", lambda e, nb=nb, k=k, yb=yb: e.matmul(P[nb][:], lhsT=yb[:, k, :], rhs=WO[:, k, nb * 512:(nb + 1) * 512], start=(k == 0), stop=(k == 15)),
                                  reads=[yb, WO], writes=[P[nb]])
                    for nb in range(4):
                        cs = slice(nb * 512, (nb + 1) * 512)
                        fw.op("dve", lambda e, nb=nb, cs=cs: e.tensor_tensor(out=t1[:, cs], in0=P[nb][:], in1=GT1[:, cs], op=ALU.mult), reads=[P[nb], GT1], writes=[t1])
                    fw.op("dve", lambda e, xb=xb: e.tensor_tensor(out=xb[:], in0=t1[:], in1=xb[:], op=ALU.add), reads=[t1, xb], writes=[xb])
                    fw.dma("sp", X1.h[ts, :], xb[:], reads=[xb], writes=[X1])
                    if cut <= 1: continue
                    fw.op("act", lambda e, xb=xb: e.activation(out=t1[:], in_=xb[:], func=AF.Square, accum_out=sm["ss"][:]), reads=[xb], writes=[t1, sm["ss"]])
                    fw.op("act", lambda e: e.activation(out=sm["rstd"][:], in_=sm["ss"][:], func=AF.Sqrt, bias=epsT[:], scale=1.0 / D), reads=[sm["ss"], epsT], writes=[sm["rstd"]])
                    fw.op("dve", lambda e: e.reciprocal(out=sm["rstd"][:], in_=sm["rstd"][:]), reads=[sm["rstd"]], writes=[sm["rstd"]])
                    fw.op("dve", lambda e, xb=xb: e.scalar_tensor_tensor(out=t2[:], in0=xb[:], scalar=sm["rstd"][:], in1=G2[:], op0=ALU.mult, op1=ALU.mult),
                          reads=[xb, sm["rstd"], G2], writes=[t2])
                    fw.op("dve", lambda e: e.tensor_tensor(out=t2[:], in0=t2[:], in1=SH2[:], op=ALU.add), reads=[t2, SH2], writes=[t2])
                    if cut <= 2: continue
                    hb = hTb[t % 2]
                    for k in range(16):
                        pb = P[4 + k // 4]
                        fw.op("pe", lambda e, k=k, pb=pb: e.transpose(out=pb[:, (k % 4) * 128:(k % 4 + 1) * 128], in_=t2[:, k * 128:(k + 1) * 128], identity=ident[:]),
                              reads=[t2, ident], writes=[pb])
                    for q in range(4):
                        pb = P[4 + q]
                        fw.op("act", lambda e, q=q, pb=pb: e.copy(out=hTf[:, q * 4:(q + 1) * 4, :], in_=pb[:].rearrange("p (a b) -> p a b", a=4)), reads=[pb], writes=[hTf])
                        fw.op("dve", lambda e, q=q, hb=hb: e.tensor_copy(out=hb[:, q * 4:(q + 1) * 4, :], in_=hTf[:, q * 4:(q + 1) * 4, :]), reads=[hTf], writes=[hb])
                    fw.dma("sp", H2T.h[:, :, ts], hb[:], reads=[hb], writes=[H2T])
                    if cut <= 3: continue
                    for k in range(16):
                        fw.op("pe", lambda e, k=k: e.matmul(P[0][:, 0:36], lhsT=hTf[:, k, :], rhs=WR[:, k, :], start=(k == 0), stop=False), reads=[hTf, WR], writes=[P[0]])
                    fw.op("pe", lambda e: e.matmul(P[0][:, 0:36], lhsT=ones[0:1, :], rhs=brr[0:1, :], start=False, stop=True), reads=[ones, brr], writes=[P[0]])
                    S = sm
                    if cut <= 4: continue
                    fw.op("act", lambda e: e.copy(out=S["lg"][:], in_=P[0][:, 0:36]), reads=[P[0]], writes=[S["lg"]])
                    fw.op("dve", lambda e: e.reduce_max(out=S["gmax"][:], in_=S["lg"][:, 0:4], axis=AX.X), reads=[S["lg"]], writes=[S["gmax"]])
                    fw.op("dve", lambda e: e.tensor_scalar(out=S["ngmax"][:], in0=S["gmax"][:], scalar1=-1.0, scalar2=None, op0=ALU.mult), reads=[S["gmax"]], writes=[S["ngmax"]])
                    fw.op("act", lambda e: e.activation(out=S["eg"][:], in_=S["lg"][:, 0:4], func=AF.Exp, bias=S["ngmax"][:], scale=1.0, accum_out=S["sumg"][:]),
                          reads=[S["lg"], S["ngmax"]], writes=[S["eg"], S["sumg"]])
                    fw.op("dve", lambda e: e.reciprocal(out=S["pgrp"][:], in_=S["sumg"][:]), reads=[S["sumg"]], writes=[S["pgrp"]])
                    fw.op("dve", lambda e: e.tensor_scalar(out=S["oh"][:], in0=S["lg"][:, 0:4], scalar1=S["gmax"][:], scalar2=None, op0=ALU.is_ge), reads=[S["lg"], S["gmax"]], writes=[S["oh"]])
                    fw.op("dve", lambda e: e.tensor_scalar(out=S["pen"][:], in0=S["oh"][:], scalar1=-1.0, scalar2=1e30, op0=ALU.add, op1=ALU.mult), reads=[S["oh"]], writes=[S["pen"]])
                    for g in range(4):
                        fw.op("dve", lambda e, g=g: e.tensor_scalar(out=S["lm"][:, g * 8:(g + 1) * 8], in0=S["lg"][:, 4 + g * 8:4 + (g + 1) * 8], scalar1=S["pen"][:, g:g + 1], scalar2=None, op0=ALU.add),
                              reads=[S["lg"], S["pen"]], writes=[S["lm"]])
                    fw.op("dve", lambda e: e.max(out=S["top8"][:], in_=S["lm"][:]), reads=[S["lm"]], writes=[S["top8"]])
                    fw.op("dve", lambda e: e.tensor_scalar(out=S["sel"][:], in0=S["lm"][:], scalar1=S["top8"][:, 1:2], scalar2=None, op0=ALU.is_ge), reads=[S["lm"], S["top8"]], writes=[S["sel"]])
                    fw.op("dve", lambda e: e.tensor_scalar(out=S["nm1"][:], in0=S["top8"][:, 0:1], scalar1=-1.0, scalar2=None, op0=ALU.mult), reads=[S["top8"]], writes=[S["nm1"]])
                    fw.op("act", lambda e: e.activation(out=S["ew"][:], in_=S["lm"][:], func=AF.Exp, bias=S["nm1"][:], scale=1.0), reads=[S["lm"], S["nm1"]], writes=[S["ew"]])
                    fw.op("dve", lambda e: e.tensor_tensor(out=S["ew"][:], in0=S["ew"][:], in1=S["sel"][:], op=ALU.mult), reads=[S["ew"], S["sel"]], writes=[S["ew"]])
                    fw.op("dve", lambda e: e.reduce_sum(out=S["den"][:], in_=S["ew"][:], axis=AX.X), reads=[S["ew"]], writes=[S["den"]])
                    fw.op("dve", lambda e: e.reciprocal(out=S["fac"][:], in_=S["den"][:]), reads=[S["den"]], writes=[S["fac"]])
                    fw.op("dve", lambda e: e.tensor_tensor(out=S["fac"][:], in0=S["fac"][:], in1=S["pgrp"][:], op=ALU.mult), reads=[S["fac"], S["pgrp"]], writes=[S["fac"]])
                    fw.op("dve", lambda e, t=t: e.tensor_scalar(out=WT[:, t, :], in0=S["ew"][:], scalar1=S["fac"][:], scalar2=None, op0=ALU.mult), reads=[S["ew"], S["fac"]], writes=[WT])
                fw.barrier()
        TP = TOK // n_pass
        NTP = TP // 128
        NG = TP // 512
        with contextlib.ExitStack() as sM:
            ACC = sbs(fw, sM, "ACC", [128, NTP, D])
            HP = sbs(fw, sM, "HP", [128, 16, TP], BF16)
            w1c = [sbs(fw, sM, "w1c%d" % i, [128, 16, 128], BF16) for i in range(2)]
            w3c = [sbs(fw, sM, "w3c%d" % i, [128, 16, 128], BF16) for i in range(2)]
            w2c = [sbs(fw, sM, "w2c%d" % i, [128, D], BF16) for i in range(8)]
            G = sbs(fw, sM, "G", [128, 6, TP], BF16)
            sl = [sbs(fw, sM, "sl%d" % i, [128, 512]) for i in range(2)]
            x1t = [sbs(fw, sM, "x1t%d" % i, [128, D]) for i in range(2)]
            ss2 = sbs(fw, sM, "ss2", [128, 1])
            rs2 = sbs(fw, sM, "rs2", [128, 1])
            ci = 0; w2i = 0; si = 0
            for p in range(n_pass):
                fw.dma("sp", HP[:], H2T.h[:, :, p * TP:(p + 1) * TP], reads=[H2T], writes=[HP])
                for ex in range(n_experts):
                    w1v = w1.h[ex].rearrange("(k p) n -> p k n", p=128)
                    w3v = w3.h[ex].rearrange("(k p) n -> p k n", p=128)
                    w2v = w2.h[ex].rearrange("(k p) n -> p k n", p=128)
                    w2l = []
                    for kc in range(6):
                        wb = w2c[w2i % 8]; w2i += 1
                        fw.dma("pool", wb[:], w2v[:, kc, :], reads=[w2], writes=[wb])
                        w2l.append(wb)
                    for cc in range(6):
                        a1, a3 = w1c[ci % 2], w3c[ci % 2]; ci += 1
                        fw.dma("pool", a1[:], w1v[:, :, cc * 128:(cc + 1) * 128], reads=[w1], writes=[a1])
                        fw.dma("pool", a3[:], w3v[:, :, cc * 128:(cc + 1) * 128], reads=[w3], writes=[a3])
                        for tg in range(NG):
                            pu1, pu3 = P[(2 * tg) % 4], P[(2 * tg + 1) % 4]
                            gs = slice(tg * 512, (tg + 1) * 512)
                            for k in range(16):
                                fw.op("pe", lambda e, k=k, a1=a1, pu1=pu1, gs=gs: e.matmul(pu1[:], lhsT=a1[:, k, :], rhs=HP[:, k, gs], start=(k == 0), stop=(k == 15)), reads=[a1, HP], writes=[pu1])
                            for k in range(16):
                                fw.op("pe", lambda e, k=k, a3=a3, pu3=pu3, gs=gs: e.matmul(pu3[:], lhsT=a3[:, k, :], rhs=HP[:, k, gs], start=(k == 0), stop=(k == 15)), reads=[a3, HP], writes=[pu3])
                            sb_ = sl[si % 2]; si += 1
                            fw.op("act", lambda e, sb_=sb_, pu1=pu1: e.activation(out=sb_[:], in_=pu1[:], func=AF.Silu), reads=[pu1], writes=[sb_])
                            fw.op("dve", lambda e, sb_=sb_, pu3=pu3, cc=cc, gs=gs: e.tensor_tensor(out=G[:, cc, gs], in0=sb_[:], in1=pu3[:], op=ALU.mult), reads=[sb_, pu3], writes=[G])
                    for tl in range(NTP):
                        tglob = p * NTP + tl
                        for nb in range(4):
                            pd = P[4 + (tl * 4 + nb) % 4]
                            cs = slice(nb * 512, (nb + 1) * 512)
                            for kc in range(6):
                                fw.op("pe", lambda e, kc=kc, pd=pd, tl=tl, cs=cs: e.matmul(pd[:], lhsT=G[:, kc, tl * 128:(tl + 1) * 128], rhs=w2l[kc][:, cs], start=(kc == 0), stop=(kc == 5)),
                                      reads=[G, w2l[kc]], writes=[pd])
                            if ex == 0:
                                fw.op("dve", lambda e, pd=pd, tl=tl, cs=cs, tglob=tglob: e.tensor_scalar(out=ACC[:, tl, cs], in0=pd[:], scalar1=WT[:, tglob, 0:1], scalar2=None, op0=ALU.mult),
                                      reads=[pd, WT], writes=[ACC])
                            else:
                                fw.op("dve", lambda e, pd=pd, tl=tl, cs=cs, tglob=tglob, ex=ex: e.scalar_tensor_tensor(out=ACC[:, tl, cs], in0=pd[:], scalar=WT[:, tglob, ex:ex + 1], in1=ACC[:, tl, cs], op0=ALU.mult, op1=ALU.add),
                                      reads=[pd, WT, ACC], writes=[ACC])
                for tl in range(NTP):
                    tglob = p * NTP + tl
                    ts = slice(tglob * 128, (tglob + 1) * 128)
                    xb = x1t[tl % 2]
                    fw.dma("sp", xb[:], X1.h[ts, :], reads=[X1], writes=[xb])
                    fw.op("dve", lambda e, tl=tl: e.tensor_tensor(out=ACC[:, tl, :], in0=ACC[:, tl, :], in1=GT2[:], op=ALU.mult), reads=[ACC, GT2], writes=[ACC])
                    fw.op("dve", lambda e, tl=tl, xb=xb: e.tensor_tensor(out=xb[:], in0=ACC[:, tl, :], in1=xb[:], op=ALU.add), reads=[ACC, xb], writes=[xb])
                    fw.op("act", lambda e, tl=tl, xb=xb: e.activation(out=ACC[:, tl, :], in_=xb[:], func=AF.Square, accum_out=ss2[:]), reads=[xb], writes=[ACC, ss2])
                    fw.op("act", lambda e: e.activation(out=rs2[:], in_=ss2[:], func=AF.Sqrt, bias=epsT[:], scale=1.0 / D), reads=[ss2, epsT], writes=[rs2])
                    fw.op("dve", lambda e: e.reciprocal(out=rs2[:], in_=rs2[:]), reads=[rs2], writes=[rs2])
                    fw.op("dve", lambda e, xb=xb: e.scalar_tensor_tensor(out=xb[:], in0=xb[:], scalar=rs2[:], in1=GFIN[:], op0=ALU.mult, op1=ALU.mult), reads=[xb, rs2, GFIN], writes=[xb])
                    fw.dma("sp", out.h[ts, :], xb[:], reads=[xb], writes=[out], is_output=True)
            fw.finish()
    return nc


D = 2048
EPS = 1e-6
NEG = -30000.0
LV = [1, 2, 4, 8, 16, 32, 64]


def l1_consts():
    p = np.arange(128)[:, None]; f = np.arange(128)[None, :]
    tri0 = (p <= f).astype(np.float32)
    ni0 = np.where(p <= f, 0.0, NEG).astype(np.float32)
    ns0 = np.where(p < f, 0.0, NEG).astype(np.float32)
    mats = [tri0, tri0.T, ni0, ni0.T, ns0, ns0.T]
    for s in LV:
        m = ((p // (2 * s) == f // (2 * s)) & (p % (2 * s) >= s) & (f % (2 * s) < s)).astype(np.float32)
        mats += [m, m.T]
    mats.append(np.eye(128, dtype=np.float32))
    return np.ascontiguousarray(np.stack(mats, axis=1))


def build_l1(NBL=64, stop=None):
    nc = bass.Bass("TRN2", target_bir_lowering=False)
    NB = NBL + 2
    with contextlib.ExitStack() as st:
        fw = FW(nc, st)
        def din(name, shape, dt=F32):
            return Tile(name, nc.dram_tensor(name, list(shape), dt, kind="ExternalInput").ap())
        x = din("x", [NBL * 128, D]); ctx = din("ctx", [256, D])
        c_l = din("c_l", [128, 16]); cc_l = din("cc_l", [128, 16])
        w_ada1 = din("w_ada1", [D, 2 * D]); b_ada1 = din("b_ada1", [1, 2 * D])
        gn1 = din("gn1", [D])
        wF = din("wF", [D, 1024]); wT = din("wT", [D, 908])
        wconv = din("wconv", [128, 6, 5])
        gbias = din("gbias", [4]); alog = din("alog", [4]); dtb = din("dtb", [4])
        ghm = din("ghm", [256]); ghd = din("ghd", [256])
        constd = din("constd", [128, 21, 128])
        y = Tile("y", nc.dram_tensor("y", [NBL * 128, 512], BF16, kind="ExternalOutput").ap())
        SM_FM = fw.dram("sm_fm", [NB, 128, 2, 128])
        SM_TM = fw.dram("sm_tm", [NB, 128, 385])
        SG_FM = fw.dram("sg_fm", [NB, 128, 4, 128])
        SG_TM = fw.dram("sg_tm", [NB, 128, 4, 128])
        SMALL = fw.dram("small", [NB, 128, 12])
        OG = fw.dram("og", [NBL, 128, 512])
        H0 = fw.dram("h0", [NBL, 128, 512])

        CONST = sbs(fw, st, "CONST", [128, 21, 128])
        fw.dma("sp", CONST[:], constd.h, reads=[constd], writes=[CONST])
        TRI = [CONST[:, 0, :], CONST[:, 1, :]]
        NI = [CONST[:, 2, :], CONST[:, 3, :]]
        NS = [CONST[:, 4, :], CONST[:, 5, :]]
        def MK(d, li): return CONST[:, 6 + 2 * li + d, :]
        def MKT(d, li): return CONST[:, 6 + 2 * li + (1 - d), :]
        ident = CONST[:, 20, :]
        ones = sbs(fw, st, "ones", [128, 128])
        epsT = sbs(fw, st, "eps", [128, 1])
        identb = sbs(fw, st, "identb", [128, 128], BF16)
        fw.op("dve", lambda e: e.memset(ones[:], 1.0), writes=[ones])
        fw.op("dve", lambda e: e.memset(epsT[:], EPS), writes=[epsT])
        fw.op("dve", lambda e: e.tensor_copy(out=identb[:], in_=ident), reads=[CONST], writes=[identb])
        P = [fw.ps("P%d" % i, [128, 512], F32) for i in range(8)]
        pbank = [0]
        def nb_():
            pbank[0] += 1
            return P[pbank[0] % 8]

        def TT(e, out, a, b, op, R, W): return fw.op(e, lambda en: en.tensor_tensor(out=out, in0=a, in1=b, op=op), reads=R, writes=W)
        def TS(e, out, a, s1, op0, R, W, s2=None, op1=None):
            if op1 is None:
                return fw.op(e, lambda en: en.tensor_scalar(out=out, in0=a, scalar1=s1, scalar2=None, op0=op0), reads=R, writes=W)
            return fw.op(e, lambda en: en.tensor_scalar(out=out, in0=a, scalar1=s1, scalar2=s2, op0=op0, op1=op1), reads=R, writes=W)
        def STT(e, out, a, s, b, op0, op1, R, W): return fw.op(e, lambda en: en.scalar_tensor_tensor(out=out, in0=a, scalar=s, in1=b, op0=op0, op1=op1), reads=R, writes=W)
        def ACT(out, in_, func, R, W, bias=None, scale=None, accum=None):
            kw = {}
            if bias is not None: kw["bias"] = bias
            if scale is not None: kw["scale"] = scale
            if accum is not None: kw["accum_out"] = accum
            return fw.op("act", lambda en: en.activation(out=out, in_=in_, func=func, **kw), reads=R, writes=W)
        def MM(out, lhsT, rhs, start, stop, R, W): return fw.op("pe", lambda en: en.matmul(out, lhsT=lhsT, rhs=rhs, start=start, stop=stop), reads=R, writes=W)
        def TR(out, in_, idn, R, W): return fw.op("pe", lambda en: en.transpose(out=out, in_=in_, identity=idn), reads=R, writes=W)
        def CP(e, out, in_, R, W):
            if e == "act":
                return fw.op("act", lambda en: en.copy(out=out, in_=in_), reads=R, writes=W)
            return fw.op(e, lambda en: en.tensor_copy(out=out, in_=in_), reads=R, writes=W)

        with contextlib.ExitStack() as sAB:
            G1 = sbs(fw, sAB, "G1", [128, D]); SH1 = sbs(fw, sAB, "SH1", [128, D])
            CG1 = sbs(fw, sAB, "CG1", [128, D]); CSH1 = sbs(fw, sAB, "CSH1", [128, D])
            with contextlib.ExitStack() as sA:
                GN1 = sbs(fw, sA, "GN1", [128, D])
                brow = sbs(fw, sA, "brow", [1, 2 * D])
                Wst = [sbs(fw, sA, "Wst%d" % i, [128, 4096]) for i in range(2)]
                fw.dma("sp", brow[:], b_ada1.h, reads=[b_ada1], writes=[brow])
                fw.dma("sp", GN1[:], gn1.h.partition_broadcast(128), reads=[gn1], writes=[GN1])
                li = 0
                for (cv, Gd, SHd) in ((c_l, G1, SH1), (cc_l, CG1, CSH1)):
                    csl = sbs(fw, sA, "csl", [128, 16]); CB = sbs(fw, sA, "CB", [128, 16, 128])
                    fw.dma("sp", csl[:], cv.h, reads=[cv], writes=[csl])
                    ACT(csl[:], csl[:], AF.Silu, [csl], [csl])
                    for k in range(16):
                        TS("dve", CB[:, k, :], ones[:], csl[:, k:k + 1], ALU.mult, [ones, csl], [CB])
                    for k in range(16):
                        wt = Wst[li % 2]; li += 1
                        fw.dma("sp", wt[:], w_ada1.h[k * 128:(k + 1) * 128, :], reads=[w_ada1], writes=[wt])
                        for j in range(8):
                            MM(P[j][:], CB[:, k, :], wt[:, j * 512:(j + 1) * 512], k == 0, False, [CB, wt], [P[j]])
                    for j in range(8):
                        MM(P[j][:], ones[0:1, :], brow[0:1, j * 512:(j + 1) * 512], False, True, [ones, brow], [P[j]])
                    for j in range(8):
                        cs = slice((j % 4) * 512, (j % 4 + 1) * 512)
                        if j < 4:
                            CP("act", SHd[:, cs], P[j][:], [P[j]], [SHd])
                        else:
                            STT("dve", Gd[:, cs], P[j][:], 1.0, GN1[:, cs], ALU.add, ALU.mult, [P[j], GN1], [Gd])
                fw.barrier()
            if stop == "A":
                for i, tl in enumerate((G1, SH1, CG1, CSH1)):
                    fw.dma("sp", y.h[i * 128:(i + 1) * 128, :].bitcast(F32) if False else H0.h[0, :, :], tl[:, 0:512], reads=[tl], writes=[H0])
                fw.finish(); return nc
            with contextlib.ExitStack() as sB:
                WFs = sbs(fw, sB, "WFs", [128, 16, 1024], BF16)
                WTs = sbs(fw, sB, "WTs", [128, 16, 908], BF16)
                WC = sbs(fw, sB, "WC", [128, 6, 5])
                fw.dma("pool", WFs[:], wF.h.rearrange("(k p) n -> p k n", p=128), reads=[wF], writes=[WFs])
                fw.dma("pool", WTs[:], wT.h.rearrange("(k p) n -> p k n", p=128), reads=[wT], writes=[WTs])
                fw.dma("sp", WC[:], wconv.h, reads=[wconv], writes=[WC])
                xt = [sbs(fw, sB, "xt%d" % i, [128, D]) for i in range(2)]
                junk = sbs(fw, sB, "junk", [128, D])
                hb = sbs(fw, sB, "hb", [128, D], BF16)
                hT = sbs(fw, sB, "hT", [128, 16, 128], BF16)
                ss = sbs(fw, sB, "ss", [128, 1]); rstd = sbs(fw, sB, "rstd", [128, 1])
                fmm = [sbs(fw, sB, "fmm%d" % i, [128, 2, 128]) for i in range(2)]
                tmm = [sbs(fw, sB, "tmm%d" % i, [128, 385]) for i in range(2)]
                ogt = [sbs(fw, sB, "ogt%d" % i, [128, 512]) for i in range(2)]
                smt = [sbs(fw, sB, "smt%d" % i, [128, 12]) for i in range(2)]
                for t_ in tmm:
                    fw.op("dve", lambda e, t_=t_: e.memset(t_[:, 384:385], 1.0), writes=[t_])
                pc = sbs(fw, sB, "pc", [128, 6, 256]); cv_ = sbs(fw, sB, "cv", [128, 6, 256])
                sq = sbs(fw, sB, "sq", [128, 4, 256]); rn = sbs(fw, sB, "rn", [128, 4, 256])
                gfm = [sbs(fw, sB, "gfm%d" % i, [128, 4, 128]) for i in range(2)]
                gtm = [sbs(fw, sB, "gtm%d" % i, [128, 4, 128]) for i in range(2)]
                Pb = [P[i][:].bitcast(BF16) for i in range(8)]
                cnt = [0]

                def project(src, row0, blk, Gm, SHm, col0, is_lat):
                    i = cnt[0]; cnt[0] += 1
                    xb = xt[i % 2]
                    fw.dma("sp", xb[:], src.h[row0:row0 + 128, :], reads=[src], writes=[xb])
                    ACT(junk[:], xb[:], AF.Square, [xb], [junk, ss], accum=ss[:])
                    ACT(rstd[:], ss[:], AF.Sqrt, [ss, epsT], [rstd], bias=epsT[:], scale=1.0 / D)
                    fw.op("dve", lambda e: e.reciprocal(out=rstd[:], in_=rstd[:]), reads=[rstd], writes=[rstd])
                    STT("dve", junk[:], xb[:], rstd[:], Gm[:], ALU.mult, ALU.mult, [xb, rstd, Gm], [junk])
                    TT("dve", hb[:], junk[:], SHm[:], ALU.add, [junk, SHm], [hb])
                    pa, pb2 = (0, 1)
                    for k in range(16):
                        bk = P[k // 8]
                        TR(Pb[k // 8][:, (k % 8) * 128:(k % 8 + 1) * 128], hb[:, k * 128:(k + 1) * 128], identb[:], [hb, identb], [bk])
                    for q in range(2):
                        CP("act" if q == 0 else "dve", hT[:, q * 8:(q + 1) * 8, :], Pb[q].rearrange("p (a b) -> p a b", a=8), [P[q]], [hT])
                    for c in range(8):
                        bk = P[2 + c // 4]
                        for k in range(16):
                            MM(bk[:, (c % 4) * 128:(c % 4 + 1) * 128], WFs[:, k, c * 128:(c + 1) * 128], hT[:, k, :], k == 0, k == 15, [WFs, hT], [bk])
                    for k in range(16):
                        MM(P[4][:], hT[:, k, :], WTs[:, k, 0:512], k == 0, k == 15, [hT, WTs], [P[4]])
                    for k in range(16):
                        MM(P[5][:, 0:396], hT[:, k, :], WTs[:, k, 512:908], k == 0, k == 15, [hT, WTs], [P[5]])
                    fm = fmm[i % 2]; tm = tmm[i % 2]; og = ogt[i % 2]; sm_ = smt[i % 2]
                    TS("dve", fm[:, 0, :], P[2][:, 0:128], 128.0 ** -0.5, ALU.mult, [P[2]], [fm])
                    CP("dve", fm[:, 1, :], P[2][:, 128:256], [P[2]], [fm])
                    fw.dma("sp", SM_FM.h[blk], fm[:], reads=[fm], writes=[SM_FM])
                    CP("act", pc[:, 0:2, col0:col0 + 128], P[2][:, 256:512].rearrange("p (a b) -> p a b", a=2), [P[2]], [pc])
                    CP("act", pc[:, 2:6, col0:col0 + 128], P[3][:].rearrange("p (a b) -> p a b", a=4), [P[3]], [pc])
                    CP("act", tm[:, 0:384], P[4][:, 0:384], [P[4]], [tm])
                    fw.dma("sp", SM_TM.h[blk], tm[:], reads=[tm], writes=[SM_TM])
                    CP("dve", sm_[:], P[5][:, 384:396], [P[5]], [sm_])
                    fw.dma("sp", SMALL.h[blk], sm_[:], reads=[sm_], writes=[SMALL])
                    if is_lat:
                        CP("act", og[:, 0:256], P[5][:, 0:256], [P[5]], [og])
                        CP("dve", og[:, 256:384], P[4][:, 384:512], [P[4]], [og])
                        CP("dve", og[:, 384:512], P[5][:, 256:384], [P[5]], [og])
                        fw.dma("sp", OG.h[blk], og[:], reads=[og], writes=[OG])

                def gdn_post(blks, W, R):
                    nr = W // R
                    for c in range(6):
                        src = pc[:, c, 0:W]; dst = cv_[:, c, 0:W]
                        TS("dve", dst, src, WC[:, c, 2:3], ALU.mult, [pc, WC], [cv_])
                        s3 = src.rearrange("p (r w) -> p r w", w=R); d3 = dst.rearrange("p (r w) -> p r w", w=R)
                        for j in (0, 1, 3, 4):
                            dd = j - 2
                            lo_o, hi_o = max(0, -dd), R - max(0, dd)
                            STT("dve", d3[:, :, lo_o:hi_o], s3[:, :, lo_o + dd:hi_o + dd], WC[:, c, j:j + 1], d3[:, :, lo_o:hi_o], ALU.mult, ALU.add, [pc, WC, cv_], [cv_])
                    ACT(cv_[:, :, 0:W], cv_[:, :, 0:W], AF.Silu, [cv_], [cv_])
                    TT("dve", sq[:, :, 0:W], cv_[:, 0:4, 0:W], cv_[:, 0:4, 0:W], ALU.mult, [cv_], [sq])
                    for a in range(4):
                        for h_ in range(W // 128):
                            bk = P[6 + (a * (W // 128) + h_) // 4]
                            off = ((a * (W // 128) + h_) % 4) * 128
                            MM(bk[:, off:off + 128], ones[:], sq[:, a, h_ * 128:(h_ + 1) * 128], True, True, [ones, sq], [bk])
                    nt = 4 * (W // 128)
                    for a in range(4):
                        for h_ in range(W // 128):
                            idx = a * (W // 128) + h_
                            bk = P[6 + idx // 4]; off = (idx % 4) * 128
                            ACT(rn[:, a, h_ * 128:(h_ + 1) * 128], bk[:, off:off + 128], AF.Sqrt, [bk, epsT], [rn], bias=epsT[:], scale=1.0)
                    fw.op("dve", lambda e: e.reciprocal(out=rn[:, :, 0:W], in_=rn[:, :, 0:W]), reads=[rn], writes=[rn])
                    STT("dve", cv_[:, 0:2, 0:W], cv_[:, 0:2, 0:W], 128.0 ** -0.5, rn[:, 0:2, 0:W], ALU.mult, ALU.mult, [cv_, rn], [cv_])
                    TT("dve", cv_[:, 2:4, 0:W], cv_[:, 2:4, 0:W], rn[:, 2:4, 0:W], ALU.mult, [cv_, rn], [cv_])
                    for bi, blk in enumerate(blks):
                        i = cnt[0]; cnt[0] += 1
                        gf = gfm[i % 2]; gt = gtm[i % 2]
                        cs = slice(bi * 128, (bi + 1) * 128)
                        CP("act", gf[:], cv_[:, 0:4, cs], [cv_], [gf])
                        fw.dma("sp", SG_FM.h[blk], gf[:], reads=[gf], writes=[SG_FM])
                        bk = nb_()
                        for a in range(4):
                            TR(bk[:, a * 128:(a + 1) * 128], cv_[:, 2 + a, cs], ident, [cv_, CONST], [bk])
                        CP("act", gt[:], bk[:].rearrange("p (a b) -> p a b", a=4), [bk], [gt])
                        fw.dma("sp", SG_TM.h[blk], gt[:], reads=[gt], writes=[SG_TM])

                project(ctx, 0, NBL, CG1, CSH1, 0, False)
                project(ctx, 128, NBL + 1, CG1, CSH1, 128, False)
                gdn_post([NBL, NBL + 1], 256, 256)
                for t in range(NBL):
                    project(x, t * 128, t, G1, SH1, 0, True)
                    gdn_post([t], 128, 64)
                fw.barrier()
        if stop == "B":
            fw.finish(); return nc, dict(SM_FM=SM_FM, SM_TM=SM_TM, SG_FM=SG_FM, SG_TM=SG_TM, SMALL=SMALL, OG=OG)
        build_l1_scan(nc, fw, st, locals())
        fw.finish()
    return nc


def build_l1_scan(nc, fw, st, L):
    NBL, NB = L["NBL"], L["NB"]
    TT, TS, STT, ACT, MM, TR, CP, nb_ = L["TT"], L["TS"], L["STT"], L["ACT"], L["MM"], L["TR"], L["CP"], L["nb_"]
    TRI, NI, NS, MK, MKT, ident, ones, epsT, CONST = L["TRI"], L["NI"], L["NS"], L["MK"], L["MKT"], L["ident"], L["ones"], L["epsT"], L["CONST"]
    SM_FM, SM_TM, SG_FM, SG_TM, SMALL, OG, H0, y = L["SM_FM"], L["SM_TM"], L["SG_FM"], L["SG_TM"], L["SMALL"], L["OG"], L["H0"], L["y"]
    gbias, alog, dtb, ghm, ghd = L["gbias"], L["alog"], L["dtb"], L["ghm"], L["ghd"]
    NST = [NS[1], NS[0]]
    with contextlib.ExitStack() as sC:
        def S_(name, shape, dt=F32): return sbs(fw, sC, name, shape, dt)
        SMA = S_("SMA", [128, NB, 12])
        for blk in range(NB):
            fw.dma("sp", SMA[:, blk, :], SMALL.h[blk], reads=[SMALL], writes=[SMA])
        GB = S_("GB", [128, 4]); AL = S_("AL", [128, 4]); DT = S_("DT", [128, 4])
        GHM = S_("GHM", [128, 256]); GHD = S_("GHD", [128, 256])
        for tl, src in ((GB, gbias), (AL, alog), (DT, dtb), (GHM, ghm), (GHD, ghd)):
            fw.dma("sp", tl[:], src.h.partition_broadcast(128), reads=[src], writes=[tl])
        MG = S_("MG", [128, 4, NB]); LF = S_("LF", [128, 2, NB])
        for c in range(4):
            TS("dve", MG[:, c, :], SMA[:, :, c], GB[:, c:c + 1], ALU.add, [SMA, GB], [MG])
        ACT(MG[:], MG[:], AF.Tanh, [MG], [MG], scale=1.0 / 15.0)
        TS("dve", MG[:], MG[:], 15.0, ALU.mult, [MG], [MG])
        for d in range(2):
            ACT(LF[:, d, :], MG[:, 2 * d + 1, :], AF.Exp, [MG], [LF], scale=-1.0)
        TS("dve", LF[:], LF[:], 1.0, ALU.add, [LF], [LF])
        ACT(LF[:], LF[:], AF.Ln, [LF], [LF])
        TS("dve", LF[:], LF[:], -1.0, ALU.mult, [LF], [LF])
        GZ = S_("GZ", [128, 4, NB]); T1 = S_("T1", [128, 4, NB]); GG = S_("GG", [128, 4, NB])
        BB = S_("BB", [128, 4, NB]); LNB = S_("LNB", [128, 4, NB]); BETA = S_("BETA", [128, 4, NB])
        NEA = S_("NEA", [128, 4])
        ACT(NEA[:], AL[:], AF.Exp, [AL], [NEA])
        TS("dve", NEA[:], NEA[:], -1.0, ALU.mult, [NEA], [NEA])
        for c in range(4):
            TS("dve", GZ[:, c, :], SMA[:, :, 4 + c], DT[:, c:c + 1], ALU.add, [SMA, DT], [GZ])
            CP("dve", BB[:, c, :], SMA[:, :, 8 + c], [SMA], [BB])
        def softplus_neg_abs(dst, src):
            TS("dve", dst, src, 0.0, ALU.abs_max, [GZ, BB], [T1, LNB])
        STT("dve", T1[:], GZ[:], -1.0, GZ[:], ALU.mult, ALU.max, [GZ], [T1])
        ACT(T1[:], T1[:], AF.Exp, [T1], [T1], scale=-1.0)
        TS("dve", T1[:], T1[:], 1.0, ALU.add, [T1], [T1])
        ACT(T1[:], T1[:], AF.Ln, [T1], [T1])
        STT("dve", T1[:], GZ[:], 0.0, T1[:], ALU.max, ALU.add, [GZ, T1], [T1])
        for c in range(4):
            TS("dve", GG[:, c, :], T1[:, c, :], NEA[:, c:c + 1], ALU.mult, [T1, NEA], [GG])
        STT("dve", LNB[:], BB[:], -1.0, BB[:], ALU.mult, ALU.max, [BB], [LNB])
        ACT(LNB[:], LNB[:], AF.Exp, [LNB], [LNB], scale=-1.0)
        TS("dve", LNB[:], LNB[:], 1.0, ALU.add, [LNB], [LNB])
        ACT(LNB[:], LNB[:], AF.Ln, [LNB], [LNB])
        STT("dve", LNB[:], BB[:], 0.0, LNB[:], ALU.min, ALU.subtract, [BB, LNB], [LNB])
        ACT(BETA[:], LNB[:], AF.Exp, [LNB], [BETA])

        def mk(names, shape=(128, 128)):
            return {n: S_(n, list(shape)) for n in names}
        fmL = [S_("fmL%d" % i, [128, 2, 128]) for i in range(2)]
        tmL = [S_("tmL%d" % i, [128, 385]) for i in range(2)]
        gfL = [S_("gfL%d" % i, [128, 4, 128]) for i in range(2)]
        gtL = [S_("gtL%d" % i, [128, 4, 128]) for i in range(2)]
        h0L = [S_("h0L%d" % i, [128, 512]) for i in range(2)]
        ogL = [S_("ogL%d" % i, [128, 512]) for i in range(2)]
        hs = [S_("hs%d" % i, [128, 512]) for i in range(2)]
        yt = [S_("yt%d" % i, [128, 512], BF16) for i in range(2)]
        jk = S_("jk", [128, 256])
        m_ = mk(["lfb", "WTm", "PT", "E0", "qe", "kw"]); m_.update(mk(["bt", "bs", "ws", "dec", "dn"], (128, 2)))
        CTs = S_("CTs", [128, 257])
        g_ = []
        for h in range(2):
            gd = mk(["gb%d" % h, "ngb%d" % h, "lb%d" % h, "DA%d" % h, "EAT%d" % h, "EA%d" % h, "AT%d" % h, "A%d" % h, "AttT%d" % h, "X%d" % h, "XT%d" % h,
                     "Bs%d" % h, "BTs%d" % h, "Y%d" % h, "Yp%d" % h, "Ru%d" % h, "Rw%d" % h, "kd%d" % h, "nwT%d" % h, "vn%d" % h, "E0%d" % h, "qg%d" % h, "S%d" % h])
            gd = {k[:-1]: v for k, v in gd.items()}
            gd.update({k: S_(k + str(h), [128, 2]) for k in ["bt", "ngc", "gl", "bw", "kds", "dec"]})
            g_.append(gd)
        ssn = S_("ssn", [128, 4]); rsn = S_("rsn", [128, 4])
        cnt = [0]

        def mlstm_block(d, blk, is_lat, fm, tm, hsb, h0t):
            li = MG[:, 2 * d, blk:blk + 1]; lf = LF[:, d, blk:blk + 1]
            qT, kT = fm[:, 0, :], fm[:, 1, :]; kTM, v1 = tm[:, 0:128], tm[:, 128:385]
            M = m_
            TS("dve", M["lfb"][:], ones[:], lf, ALU.mult, [ones, LF], [M["lfb"]])
            b0 = nb_()
            MM(b0[:, 0:128], M["lfb"][:], TRI[d], True, True, [M["lfb"], CONST], [b0])
            MM(b0[:, 128:129], TRI[d], lf, True, True, [CONST, LF], [b0])
            MM(b0[:, 129:130], ones[:], lf, True, True, [ones, LF], [b0])
            CP("dve", M["bt"][:], b0[:, 128:130], [b0], [M["bt"]])
            TT("dve", M["bs"][:, 0:1], li, M["bt"][:, 0:1], ALU.subtract, [MG, M["bt"]], [M["bs"]])
            if is_lat:
                b1 = nb_()
                MM(b1[:, 0:128], M["lfb"][:], TRI[d], True, False, [M["lfb"], CONST], [b1])
                MM(b1[:, 0:128], ident, NI[d], False, True, [CONST], [b1])
                ACT(M["WTm"][:], b1[:, 0:128], AF.Exp, [b1, M["bs"]], [M["WTm"]], bias=M["bs"][:, 0:1])
                b2 = nb_()
                MM(b2[:, 0:128], kT, qT, True, True, [fm], [b2])
                TT("dve", M["PT"][:], b2[:, 0:128], M["WTm"][:], ALU.mult, [b2, M["WTm"]], [M["PT"]])
                ACT(M["E0"][:], b0[:, 0:128], AF.Exp, [b0], [M["E0"]])
                TT("dve", M["qe"][:], qT, M["E0"][:], ALU.mult, [fm, M["E0"]], [M["qe"]])
                b3 = nb_()
                MM(b3[:, 0:257], M["PT"][:], v1, True, False, [M["PT"], tm], [b3])
                MM(b3[:, 0:257], M["qe"][:], CTs[:], False, True, [M["qe"], CTs], [b3])
                TS("dve", M["dn"][:, 1:2], b3[:, 256:257], -1.0, ALU.mult, [b3], [M["dn"]], s2=1.0, op1=ALU.max)
                TS("dve", M["dn"][:, 0:1], b3[:, 256:257], 1.0, ALU.max, [b3], [M["dn"]])
                TT("dve", M["dn"][:, 0:1], M["dn"][:, 0:1], M["dn"][:, 1:2], ALU.max, [M["dn"]], [M["dn"]])
                fw.op("dve", lambda e: e.reciprocal(out=M["dn"][:, 0:1], in_=M["dn"][:, 0:1]), reads=[M["dn"]], writes=[M["dn"]])
                if d == 0:
                    TS("dve", hsb[:, 0:256], b3[:, 0:256], M["dn"][:, 0:1], ALU.mult, [b3, M["dn"]], [hsb])
                else:
                    STT("dve", hsb[:, 0:256], b3[:, 0:256], M["dn"][:, 0:1], h0t[:, 0:256], ALU.mult, ALU.add, [b3, M["dn"], h0t], [hsb])
            ACT(M["ws"][:, 0:1], M["bs"][:, 0:1], AF.Exp, [M["bs"], M["bt"]], [M["ws"]], bias=M["bt"][:, 1:2])
            TS("dve", M["kw"][:], kTM, M["ws"][:, 0:1], ALU.mult, [tm, M["ws"]], [M["kw"]])
            b4 = nb_()
            MM(b4[:, 0:257], M["kw"][:], v1, True, True, [M["kw"], tm], [b4])
            ACT(M["dec"][:, 0:1], M["bt"][:, 1:2], AF.Exp, [M["bt"]], [M["dec"]])
            STT("dve", CTs[:], CTs[:], M["dec"][:, 0:1], b4[:, 0:257], ALU.mult, ALU.add, [CTs, M["dec"], b4], [CTs])

        def gdn_block(d, h, blk, is_lat, gf, gt, hsb, h0t):
            G = g_[h]; c = 2 * d + h
            g = GG[:, c, blk:blk + 1]; lnb = LNB[:, c, blk:blk + 1]; beta = BETA[:, c, blk:blk + 1]
            qT, kT, kTM, vTM = gf[:, h, :], gf[:, 2 + h, :], gt[:, h, :], gt[:, 2 + h, :]
            TS("dve", G["gb"][:], ones[:], g, ALU.mult, [ones, GG], [G["gb"]])
            TS("dve", G["ngb"][:], G["gb"][:], -1.0, ALU.mult, [G["gb"]], [G["ngb"]])
            TS("dve", G["lb"][:], ones[:], lnb, ALU.mult, [ones, LNB], [G["lb"]])
            b0 = nb_()
            MM(b0[:, 0:128], G["gb"][:], TRI[d], True, True, [G["gb"], CONST], [b0])
            MM(b0[:, 128:129], TRI[d], g, True, True, [CONST, GG], [b0])
            MM(b0[:, 129:130], ones[:], g, True, True, [ones, GG], [b0])
            CP("dve", G["bt"][:], b0[:, 128:130], [b0], [G["bt"]])
            if is_lat:
                ACT(G["E0"][:], b0[:, 0:128], AF.Exp, [b0], [G["E0"]])
            TS("dve", G["ngc"][:, 0:1], G["bt"][:, 0:1], -1.0, ALU.mult, [G["bt"]], [G["ngc"]])
            TT("dve", G["gl"][:, 0:1], G["bt"][:, 0:1], lnb, ALU.add, [G["bt"], LNB], [G["gl"]])
            if is_lat:
                b1 = nb_()
                MM(b1[:, 0:128], G["gb"][:], TRI[d], True, False, [G["gb"], CONST], [b1])
                MM(b1[:, 0:128], ident, NI[d], False, True, [CONST], [b1])
                ACT(G["DA"][:], b1[:, 0:128], AF.Exp, [b1, G["ngc"]], [G["DA"]], bias=G["ngc"][:, 0:1])
            b2 = nb_()
            MM(b2[:, 0:128], G["gb"][:], TRI[d], True, False, [G["gb"], CONST], [b2])
            MM(b2[:, 0:128], G["lb"][:], ident, False, False, [G["lb"], CONST], [b2])
            MM(b2[:, 0:128], ident, NS[d], False, True, [CONST], [b2])
            ACT(G["EAT"][:], b2[:, 0:128], AF.Exp, [b2, G["ngc"]], [G["EAT"]], bias=G["ngc"][:, 0:1])
            b3 = nb_()
            MM(b3[:, 0:128], G["ngb"][:], TRI[d], True, False, [G["ngb"], CONST], [b3])
            MM(b3[:, 0:128], ident, NST[d], False, True, [CONST], [b3])
            ACT(G["EA"][:], b3[:, 0:128], AF.Exp, [b3, G["gl"]], [G["EA"]], bias=G["gl"][:, 0:1])
            b4 = nb_()
            MM(b4[:, 0:128], kT, kT, True, True, [gf], [b4])
            TT("dve", G["AT"][:], b4[:, 0:128], G["EAT"][:], ALU.mult, [b4, G["EAT"]], [G["AT"]])
            TT("dve", G["A"][:], b4[:, 0:128], G["EA"][:], ALU.mult, [b4, G["EA"]], [G["A"]])
            if is_lat:
                b5 = nb_()
                MM(b5[:, 0:128], kT, qT, True, True, [gf], [b5])
                TT("dve", G["AttT"][:], b5[:, 0:128], G["DA"][:], ALU.mult, [b5, G["DA"]], [G["AttT"]])
            TT("dve", G["Bs"][:], G["A"][:], MK(d, 0), ALU.mult, [G["A"], CONST], [G["Bs"]])
            TT("dve", G["X"][:], ident, G["Bs"][:], ALU.subtract, [CONST, G["Bs"]], [G["X"]])
            TT("dve", G["BTs"][:], G["AT"][:], MKT(d, 0), ALU.mult, [G["AT"], CONST], [G["BTs"]])
            TT("dve", G["XT"][:], ident, G["BTs"][:], ALU.subtract, [CONST, G["BTs"]], [G["XT"]])
            for li in range(1, 7):
                last = li == 6
                TT("dve", G["Bs"][:], G["A"][:], MK(d, li), ALU.mult, [G["A"], CONST], [G["Bs"]])
                pyp = nb_()
                MM(pyp[:, 0:128], G["Bs"][:], G["XT"][:], True, True, [G["Bs"], G["XT"]], [pyp])
                CP("act", G["Yp"][:], pyp[:, 0:128], [pyp], [G["Yp"]])
                pzp = nb_()
                MM(pzp[:, 0:128], G["X"][:], G["Yp"][:], True, True, [G["X"], G["Yp"]], [pzp])
                if not last:
                    TT("dve", G["BTs"][:], G["AT"][:], MKT(d, li), ALU.mult, [G["AT"], CONST], [G["BTs"]])
                    py = nb_()
                    MM(py[:, 0:128], G["BTs"][:], G["X"][:], True, True, [G["BTs"], G["X"]], [py])
                    CP("act", G["Y"][:], py[:, 0:128], [py], [G["Y"]])
                    pz = nb_()
                    MM(pz[:, 0:128], G["XT"][:], G["Y"][:], True, True, [G["XT"], G["Y"]], [pz])
                    TT("dve", G["X"][:], G["X"][:], pz[:, 0:128], ALU.subtract, [G["X"], pz], [G["X"]])
                TT("dve", G["XT"][:], G["XT"][:], pzp[:, 0:128], ALU.subtract, [G["XT"], pzp], [G["XT"]])
            ACT(G["bw"][:, 0:1], G["gl"][:, 0:1], AF.Exp, [G["gl"]], [G["bw"]])
            TS("dve", G["Ru"][:], vTM, beta, ALU.mult, [gt, BETA], [G["Ru"]])
            TS("dve", G["Rw"][:], kTM, G["bw"][:, 0:1], ALU.mult, [gt, G["bw"]], [G["Rw"]])
            ACT(G["kds"][:, 0:1], G["bt"][:, 0:1], AF.Exp, [G["bt"]], [G["kds"]], scale=-1.0, bias=G["bt"][:, 1:2])
            TS("dve", G["kd"][:], kTM, G["kds"][:, 0:1], ALU.mult, [gt, G["kds"]], [G["kd"]])
            pw = nb_()
            MM(pw[:, 0:128], G["Rw"][:], G["XT"][:], True, True, [G["Rw"], G["XT"]], [pw])
            fw.op("act", lambda e: e.mul(out=G["nwT"][:], in_=pw[:, 0:128], mul=-1.0), reads=[pw], writes=[G["nwT"]])
            pv = nb_()
            MM(pv[:, 0:128], G["XT"][:], G["Ru"][:], True, False, [G["XT"], G["Ru"]], [pv])
            MM(pv[:, 0:128], G["nwT"][:], G["S"][:], False, True, [G["nwT"], G["S"]], [pv])
            CP("act", G["vn"][:], pv[:, 0:128], [pv], [G["vn"]])
            if is_lat:
                TT("dve", G["qg"][:], qT, G["E0"][:], ALU.mult, [gf, G["E0"]], [G["qg"]])
                po = nb_()
                MM(po[:, 0:128], G["qg"][:], G["S"][:], True, False, [G["qg"], G["S"]], [po])
                MM(po[:, 0:128], G["AttT"][:], G["vn"][:], False, True, [G["AttT"], G["vn"]], [po])
                cs = slice(256 + h * 128, 256 + (h + 1) * 128)
                if d == 0:
                    CP("dve", hsb[:, cs], po[:, 0:128], [po], [hsb])
                else:
                    TT("dve", hsb[:, cs], po[:, 0:128], h0t[:, cs], ALU.add, [po, h0t], [hsb])
            pu = nb_()
            MM(pu[:, 0:128], G["kd"][:], G["vn"][:], True, True, [G["kd"], G["vn"]], [pu])
            ACT(G["dec"][:, 0:1], G["bt"][:, 1:2], AF.Exp, [G["bt"]], [G["dec"]])
            STT("dve", G["S"][:], G["S"][:], G["dec"][:, 0:1], pu[:, 0:128], ALU.mult, ALU.add, [G["S"], G["dec"], pu], [G["S"]])

        for d in range(2):
            fw.op("dve", lambda e: e.memset(CTs[:], 0.0), writes=[CTs])
            for h in range(2):
                fw.op("dve", lambda e, h=h: e.memset(g_[h]["S"][:], 0.0), writes=[g_[h]["S"]])
            order = [NBL, NBL + 1] + list(range(NBL)) if d == 0 else [NBL + 1, NBL] + list(range(NBL - 1, -1, -1))
            for blk in order:
                i = cnt[0]; cnt[0] += 1
                is_lat = blk < NBL
                fm, tm, gf, gt = fmL[i % 2], tmL[i % 2], gfL[i % 2], gtL[i % 2]
                hsb, h0t, og, yb = hs[i % 2], h0L[i % 2], ogL[i % 2], yt[i % 2]
                fw.dma("sp", fm[:], SM_FM.h[blk], reads=[SM_FM], writes=[fm])
                fw.dma("sp", tm[:], SM_TM.h[blk], reads=[SM_TM], writes=[tm])
                fw.dma("sp", gf[:], SG_FM.h[blk], reads=[SG_FM], writes=[gf])
                fw.dma("sp", gt[:], SG_TM.h[blk], reads=[SG_TM], writes=[gt])
                if is_lat and d == 1:
                    fw.dma("sp", h0t[:], H0.h[blk], reads=[H0], writes=[h0t])
                    fw.dma("sp", og[:], OG.h[blk], reads=[OG], writes=[og])
                mlstm_block(d, blk, is_lat, fm, tm, hsb, h0t)
                gdn_block(d, 0, blk, is_lat, gf, gt, hsb, h0t)
                gdn_block(d, 1, blk, is_lat, gf, gt, hsb, h0t)
                if not is_lat:
                    continue
                if d == 0:
                    fw.dma("sp", H0.h[blk], hsb[:], reads=[hsb], writes=[H0])
                    continue
                segs = [(0, 256, GHM[:, 0:256]), (256, 384, GHD[:, 0:128]), (384, 512, GHD[:, 128:256])]
                for si, (lo, hi, gsc) in enumerate(segs):
                    ACT(jk[:, 0:hi - lo], hsb[:, lo:hi], AF.Square, [hsb], [jk, ssn], accum=ssn[:, si:si + 1])
                    ACT(rsn[:, si:si + 1], ssn[:, si:si + 1], AF.Sqrt, [ssn, epsT], [rsn], bias=epsT[:], scale=1.0 / (hi - lo))
                fw.op("dve", lambda e: e.reciprocal(out=rsn[:, 0:3], in_=rsn[:, 0:3]), reads=[rsn], writes=[rsn])
                ACT(og[:, 0:256], og[:, 0:256], AF.Sigmoid, [og], [og])
                ACT(og[:, 256:512], og[:, 256:512], AF.Silu, [og], [og])
                for si, (lo, hi, gsc) in enumerate(segs):
                    STT("dve", hsb[:, lo:hi], hsb[:, lo:hi], rsn[:, si:si + 1], gsc, ALU.mult, ALU.mult, [hsb, rsn, GHM, GHD], [hsb])
                TT("dve", yb[:], hsb[:], og[:], ALU.mult, [hsb, og], [yb])
                fw.dma("sp", y.h[blk * 128:(blk + 1) * 128, :], yb[:], reads=[yb], writes=[y], is_output=True)

M_QK, M_V, G_W = 512, 1024, 1024
OFF = {}
o = 0
for nm, w in [("mq", 512), ("mk", 512), ("mv", 1024), ("mo", 1024), ("mg", 16), ("gqkv", 3072), ("go", 1024), ("ga", 16), ("gb", 16)]:
    OFF[nm] = o; o += w

def l1_inputs(inp, core, NBL=64):
    b, hg = core // 4, core % 4
    hm = hg; h0, h1 = 2 * hg, 2 * hg + 1
    w_in = inp["w_in"][0]
    def cols(base, start, n): return list(range(OFF[base] + start, OFF[base] + start + n))
    gq = lambda h: cols("gqkv", h * 128, 128)
    gk = lambda h: cols("gqkv", G_W + h * 128, 128)
    gv = lambda h: cols("gqkv", 2 * G_W + h * 128, 128)
    fcols = cols("mq", hm * 128, 128) + cols("mk", hm * 128, 128) + gq(h0) + gq(h1) + gk(h0) + gk(h1) + gv(h0) + gv(h1)
    mg = [OFF["mg"] + d * 8 + t * 4 + hm for d in range(2) for t in range(2)]
    ga = [OFF["ga"] + d * 8 + h for d in range(2) for h in (h0, h1)]
    gb = [OFF["gb"] + d * 8 + h for d in range(2) for h in (h0, h1)]
    tcols = cols("mk", hm * 128, 128) + cols("mv", hm * 256, 256) + cols("go", h0 * 128, 128) + cols("mo", hm * 256, 256) + cols("go", h1 * 128, 128) + mg + ga + gb
    wc = inp["w_conv"][0]
    ccols = [c - OFF["gqkv"] for c in gq(h0) + gq(h1) + gk(h0) + gk(h1) + gv(h0) + gv(h1)]
    wconv = np.ascontiguousarray(wc[:, ccols].reshape(5, 6, 128).transpose(2, 1, 0))
    bg = inp["b_gate_m"][0]
    return {
        "x": np.ascontiguousarray(inp["x"][b, :NBL * 128]), "ctx": np.ascontiguousarray(inp["ctx"][b]),
        "c_l": np.ascontiguousarray(inp["c"][b].reshape(16, 128).T), "cc_l": np.ascontiguousarray(inp["c_ctx"].reshape(16, 128).T),
        "w_ada1": np.ascontiguousarray(inp["w_ada"][0][:, :4096]), "b_ada1": np.ascontiguousarray(inp["b_ada"][0][None, :4096]),
        "gn1": inp["g_norm1"][0],
        "wF": np.ascontiguousarray(w_in[:, fcols]), "wT": np.ascontiguousarray(w_in[:, tcols]),
        "wconv": wconv,
        "gbias": np.ascontiguousarray(np.array([bg[d, t, hm] for d in range(2) for t in range(2)], np.float32)),
        "alog": np.ascontiguousarray(np.array([inp["a_log"][0][d, h] for d in range(2) for h in (h0, h1)], np.float32)),
        "dtb": np.ascontiguousarray(np.array([inp["dt_bias"][0][d, h] for d in range(2) for h in (h0, h1)], np.float32)),
        "ghm": np.ascontiguousarray(inp["g_head_m"][0][hm * 256:(hm + 1) * 256]),
        "ghd": np.ascontiguousarray(inp["g_head_d"][0][h0 * 128:(h1 + 1) * 128]),
        "constd": l1_consts(),
    }

def ycols(core):
    hg = core % 4
    return list(range(hg * 256, (hg + 1) * 256)) + list(range(1024 + 2 * hg * 128, 1024 + (2 * hg + 2) * 128))


def l2_inputs(inp, ycat, core):
    b, j = core // 4, core % 4
    ts = slice(j * 2048, (j + 1) * 2048)
    return {
        "x": np.ascontiguousarray(inp["x"][b, ts]),
        "yT": np.ascontiguousarray(ycat[b, ts].T),
        "c_l": np.ascontiguousarray(inp["c"][b].reshape(16, 128).T),
        "w_ada2": np.ascontiguousarray(inp["w_ada"][0][:, 2 * 2048:]),
        "b_ada2": np.ascontiguousarray(inp["b_ada"][0][None, 2 * 2048:]),
        "gn2": inp["g_norm2"][0], "gfin": inp["g_final"],
        "w_out": inp["w_out"][0],
        "wr": np.ascontiguousarray(np.concatenate([inp["w_grp"][0], inp["w_rtr"][0]], axis=1)),
        "br": np.ascontiguousarray(np.concatenate([inp["b_grp"][0], inp["b_rtr"][0]])[None, :]),
        "w1": inp["w1"][0], "w3": inp["w3"][0], "w2": inp["w2"][0],
        "identd": np.eye(128, dtype=np.float32),
    }


def _launch(nc, maps):
    return run_bass_kernel_spmd(nc, maps, core_ids=list(range(len(maps)))).results


def kernel(**inputs):
    inp = {k: np.asarray(v) for k, v in inputs.items()}
    nc1 = build_l1(NBL=64)
    r1 = _launch(nc1, [l1_inputs(inp, c, 64) for c in range(8)])
    ycat = np.zeros((2, 8192, 2048), dtype=ml_dtypes.bfloat16)
    for c in range(8):
        ycat[c // 4][:, ycols(c)] = r1[c]["y"]
    nc2 = build_l2()
    r2 = _launch(nc2, [l2_inputs(inp, ycat, c) for c in range(8)])
    out = np.zeros((2, 8192, 2048), dtype=np.float32)
    for c in range(8):
        out[c // 4, (c % 4) * 2048:(c % 4 + 1) * 2048] = r2[c]["out"]
    return out
```
